# Optimizing a Trainium2 kernel written in Bass

```python
import math
import jax, jax.numpy as jnp
from jax import lax
import numpy as np

D_MODEL = 2048
BATCH = 2
SEQ = 8192
DEPTH = 1

BLOCK = 128
N_META = 16
PAD = BLOCK - N_META
HEAD_DIM = 64
ATTN_WIDTH = D_MODEL // 2
N_Q_HEADS = ATTN_WIDTH // HEAD_DIM
N_KV_HEADS = N_Q_HEADS // 8
Q_PER_KV = N_Q_HEADS // N_KV_HEADS
KV_WIDTH = N_KV_HEADS * HEAD_DIM
WINDOW = 128
POOL_WINDOWS = (2, 4, 8, 16)
N_POOL_GROUPS = len(POOL_WINDOWS)
POOL_WIDTH = D_MODEL // 2
POOL_GROUP_WIDTH = POOL_WIDTH // N_POOL_GROUPS
N_BUCKETS = 32
MAX_DISTANCE = 128
N_GROUPS = 8
EXPERTS_PER_GROUP = 8
N_EXPERTS = N_GROUPS * EXPERTS_PER_GROUP
TOP_K = 2
D_EXPERT = D_MODEL // 4
MOE_BLOCK = 128
RMS_EPS = 1e-6
IN_WIDTH = ATTN_WIDTH + 2 * KV_WIDTH + POOL_WIDTH + 2 * D_MODEL

kernel_name = "hybrid_swa_pool_hmoe_block"


def rms_norm(x, g):
    xf = x.astype(jnp.float32)
    y = xf * lax.rsqrt(jnp.mean(xf * xf, axis=-1, keepdims=True) + RMS_EPS)
    return (y * g.astype(jnp.float32)).astype(x.dtype)


def t5_bucket(dist):
    max_exact = N_BUCKETS // 2
    d = jnp.maximum(dist, 0)
    large = max_exact + (jnp.log(jnp.maximum(d, 1).astype(jnp.float32) / max_exact)
                         / math.log(MAX_DISTANCE / max_exact) * (N_BUCKETS - max_exact)).astype(jnp.int32)
    large = jnp.minimum(large, N_BUCKETS - 1)
    return jnp.where(d < max_exact, d, large)


def sliding_window_attention(q, k, v, sinks, rel_bias):
    b, lp = q.shape[:2]
    nb = lp // BLOCK
    scale = HEAD_DIM ** -0.5
    qb = q.reshape(b, nb, BLOCK, N_KV_HEADS, Q_PER_KV, HEAD_DIM)
    kb = k.reshape(b, nb, BLOCK, N_KV_HEADS, HEAD_DIM)
    vb = v.reshape(b, nb, BLOCK, N_KV_HEADS, HEAD_DIM)
    zk = jnp.zeros_like(kb[:, :1])
    kwin = jnp.concatenate([jnp.concatenate([zk, kb[:, :-1]], axis=1), kb], axis=2)
    vwin = jnp.concatenate([jnp.concatenate([zk, vb[:, :-1]], axis=1), vb], axis=2)
    kmeta = k[:, PAD:BLOCK]
    vmeta = v[:, PAD:BLOCK]

    blk = jnp.arange(nb)[:, None, None]
    qi = jnp.arange(BLOCK)
    kj = jnp.arange(2 * BLOCK)
    q_pos = blk * BLOCK + qi[None, :, None]
    k_pos = (blk - 1) * BLOCK + kj[None, None, :]
    dist = q_pos - k_pos
    win_ok = (dist >= 0) & (dist < WINDOW) & (k_pos >= PAD)
    dist_local = qi[:, None] + BLOCK - kj[None, :]
    rb = rel_bias.astype(jnp.float32)
    win_bias = rb[t5_bucket(dist_local)].reshape(BLOCK, 2 * BLOCK, N_KV_HEADS, Q_PER_KV).transpose(2, 3, 0, 1)
    m_pos = PAD + jnp.arange(N_META)
    mdist = q_pos - m_pos[None, None, :]
    meta_ok = mdist >= WINDOW
    meta_bias = rb[t5_bucket(mdist)].reshape(nb, BLOCK, N_META, N_KV_HEADS, Q_PER_KV).transpose(0, 3, 4, 1, 2)

    s_win = jnp.einsum('bnqhgd,bnkhd->bnhgqk', qb, kwin).astype(jnp.float32) * scale + win_bias[None, None]
    s_win = jnp.where(win_ok[None, :, None, None], s_win, -jnp.inf)
    s_meta = jnp.einsum('bnqhgd,bmhd->bnhgqm', qb, kmeta).astype(jnp.float32) * scale + meta_bias[None]
    s_meta = jnp.where(meta_ok[None, :, None, None], s_meta, -jnp.inf)
    sink = sinks.astype(jnp.float32).reshape(N_KV_HEADS, Q_PER_KV)[None, None, :, :, None, None]
    mx = jnp.maximum(jnp.maximum(s_win.max(-1, keepdims=True), s_meta.max(-1, keepdims=True)), sink)
    p_win = jnp.exp(s_win - mx)
    p_meta = jnp.exp(s_meta - mx)
    denom = p_win.sum(-1, keepdims=True) + p_meta.sum(-1, keepdims=True) + jnp.exp(sink - mx)
    p_win = (p_win / denom).astype(v.dtype)
    p_meta = (p_meta / denom).astype(v.dtype)
    o = (jnp.einsum('bnhgqk,bnkhd->bnqhgd', p_win, vwin)
         + jnp.einsum('bnhgqm,bmhd->bnqhgd', p_meta, vmeta))
    return o.reshape(b, lp, ATTN_WIDTH)


def multiscale_pool(u, valid, w_group, scale):
    b, lp, _ = u.shape
    ug = (u.astype(jnp.float32) * valid[None, :, None]).reshape(b, lp, N_POOL_GROUPS, POOL_GROUP_WIDTH)
    c = jnp.cumsum(ug, axis=1)
    cnt = jnp.cumsum(valid.astype(jnp.float32))
    outs = []
    for gi, w in enumerate(POOL_WINDOWS):
        cp = jnp.pad(c[:, :, gi], ((0, 0), (w, 0), (0, 0)))
        win_sum = cp[:, w:] - cp[:, :lp]
        cntp = jnp.pad(cnt, (w, 0))
        n_valid = jnp.maximum(cntp[w:] - cntp[:lp], 1.0)
        outs.append(win_sum / n_valid[None, :, None] - ug[:, :, gi])
    mixed = jnp.stack(outs, axis=2).astype(u.dtype)
    y = jnp.einsum('bsgc,gcd->bsgd', mixed, w_group)
    return y.reshape(b, lp, POOL_WIDTH) * scale


def hierarchical_moe(x, w_rg, b_rg, w_re, b_re, w_gate, w_up, w_down):
    n, d = x.shape
    xf = x.astype(jnp.float32)
    g_logits = xf @ w_rg.astype(jnp.float32) + b_rg.astype(jnp.float32)
    g_prob = jax.nn.softmax(g_logits, axis=-1)
    grp = jnp.argmax(g_logits, axis=-1)
    p_grp = jnp.take_along_axis(g_prob, grp[:, None], axis=1)[:, 0]
    e_logits = (xf @ w_re.astype(jnp.float32) + b_re.astype(jnp.float32)).reshape(n, N_GROUPS, EXPERTS_PER_GROUP)
    e_in_grp = jnp.take_along_axis(e_logits, grp[:, None, None], axis=1)[:, 0]
    top_l, top_i = lax.top_k(e_in_grp, TOP_K)
    top_w = jax.nn.softmax(top_l, axis=-1) * p_grp[:, None]
    expert = grp[:, None] * EXPERTS_PER_GROUP + top_i

    a = n * TOP_K
    e_flat = expert.reshape(a)
    w_flat = top_w.reshape(a)
    tok = jnp.repeat(jnp.arange(n, dtype=jnp.int32), TOP_K)
    order = jnp.argsort(e_flat)
    e_sorted = e_flat[order]
    counts = jnp.bincount(e_flat, length=N_EXPERTS)
    starts = jnp.cumsum(counts) - counts
    padded = (counts + MOE_BLOCK - 1) // MOE_BLOCK * MOE_BLOCK
    pad_ends = jnp.cumsum(padded)
    pad_starts = pad_ends - padded
    dest = pad_starts[e_sorted] + (jnp.arange(a) - starts[e_sorted])
    n_blocks = a // MOE_BLOCK + N_EXPERTS
    slots = n_blocks * MOE_BLOCK
    slot_tok = jnp.full((slots,), n, jnp.int32).at[dest].set(tok[order])
    slot_w = jnp.zeros((slots,), x.dtype).at[dest].set(w_flat[order].astype(x.dtype))
    block_expert = jnp.minimum(jnp.searchsorted(pad_ends, jnp.arange(n_blocks) * MOE_BLOCK, side='right'),
                               N_EXPERTS - 1).astype(jnp.int32)
    x_pad = jnp.concatenate([x, jnp.zeros((1, d), x.dtype)], axis=0)

    def run_block(args):
        e, toks, ws = args
        xb = x_pad[toks]
        hb = jax.nn.silu(xb @ w_gate[e]) * (xb @ w_up[e])
        return (hb @ w_down[e]) * ws[:, None]

    yb = lax.map(run_block, (block_expert, slot_tok.reshape(n_blocks, MOE_BLOCK),
                             slot_w.reshape(n_blocks, MOE_BLOCK)))
    y = jax.ops.segment_sum(yb.reshape(slots, d), slot_tok, num_segments=n + 1)
    return y[:n]


def setup_inputs(seed: int = 0) -> dict:
    key = jax.random.key(seed)
    ks = jax.random.split(key, 20)

    def nrm(k, shape, s):
        return jax.random.normal(k, shape, jnp.float32) * s

    return {
        "x": nrm(ks[0], (BATCH, SEQ, D_MODEL), 1.0),
        "meta_tokens": nrm(ks[1], (N_META, D_MODEL), 1.0),
        "rel_bias": nrm(ks[2], (N_BUCKETS, N_Q_HEADS), 0.5),
        "norm_mix": 1.0 + nrm(ks[3], (DEPTH, D_MODEL), 0.02),
        "w_in": nrm(ks[4], (DEPTH, D_MODEL, IN_WIDTH), D_MODEL ** -0.5),
        "attn_sinks": nrm(ks[5], (DEPTH, N_Q_HEADS), 0.5),
        "pool_mix": nrm(ks[6], (DEPTH, N_POOL_GROUPS, POOL_GROUP_WIDTH, POOL_GROUP_WIDTH), POOL_GROUP_WIDTH ** -0.5),
        "pool_scale": 1.0 + nrm(ks[7], (DEPTH, POOL_WIDTH), 0.02),
        "w_attn_branch": nrm(ks[8], (DEPTH, ATTN_WIDTH, D_MODEL), ATTN_WIDTH ** -0.5),
        "w_pool_branch": nrm(ks[9], (DEPTH, POOL_WIDTH, D_MODEL), POOL_WIDTH ** -0.5),
        "w_out": nrm(ks[10], (DEPTH, D_MODEL, D_MODEL), D_MODEL ** -0.5),
        "norm_ffn": 1.0 + nrm(ks[11], (DEPTH, D_MODEL), 0.02),
        "w_router_group": nrm(ks[12], (DEPTH, D_MODEL, N_GROUPS), D_MODEL ** -0.5),
        "b_router_group": nrm(ks[13], (DEPTH, N_GROUPS), 0.01),
        "w_router_expert": nrm(ks[14], (DEPTH, D_MODEL, N_EXPERTS), D_MODEL ** -0.5),
        "b_router_expert": nrm(ks[15], (DEPTH, N_EXPERTS), 0.01),
        "w_gate": nrm(ks[16], (DEPTH, N_EXPERTS, D_MODEL, D_EXPERT), D_MODEL ** -0.5),
        "w_up": nrm(ks[17], (DEPTH, N_EXPERTS, D_MODEL, D_EXPERT), D_MODEL ** -0.5),
        "w_down": nrm(ks[18], (DEPTH, N_EXPERTS, D_EXPERT, D_MODEL), D_EXPERT ** -0.5),
        "norm_final": 1.0 + nrm(ks[19], (D_MODEL,), 0.02),
    }


def reference(x, meta_tokens, rel_bias, norm_mix, w_in, attn_sinks, pool_mix, pool_scale,
              w_attn_branch, w_pool_branch, w_out, norm_ffn, w_router_group, b_router_group,
              w_router_expert, b_router_expert, w_gate, w_up, w_down, norm_final):
    b, s, d = x.shape
    lead = jnp.concatenate([jnp.zeros((PAD, d), x.dtype), meta_tokens.astype(x.dtype)], axis=0)
    h = jnp.concatenate([jnp.broadcast_to(lead[None], (b, BLOCK, d)), x], axis=1)
    lp = s + BLOCK
    valid = jnp.arange(lp) >= PAD
    cuts = np.cumsum([ATTN_WIDTH, KV_WIDTH, KV_WIDTH, POOL_WIDTH, D_MODEL]).tolist()
    for layer in range(DEPTH):
        hn = rms_norm(h, norm_mix[layer])
        proj = hn @ w_in[layer]
        q, k, v, u, g_a, g_p = jnp.split(proj, cuts, axis=-1)
        attn = sliding_window_attention(q.reshape(b, lp, N_Q_HEADS, HEAD_DIM),
                                        k.reshape(b, lp, N_KV_HEADS, HEAD_DIM),
                                        v.reshape(b, lp, N_KV_HEADS, HEAD_DIM),
                                        attn_sinks[layer], rel_bias)
        pool = multiscale_pool(u, valid, pool_mix[layer], pool_scale[layer])
        merged = (jax.nn.sigmoid(g_a) * (attn @ w_attn_branch[layer])
                  + jax.nn.sigmoid(g_p) * (pool @ w_pool_branch[layer]))
        h = h + merged @ w_out[layer]

        hv = h[:, PAD:]
        hn2 = rms_norm(hv, norm_ffn[layer]).reshape(b * (lp - PAD), d)
        ffn = hierarchical_moe(hn2, w_router_group[layer], b_router_group[layer],
                               w_router_expert[layer], b_router_expert[layer],
                               w_gate[layer], w_up[layer], w_down[layer])
        h = h.at[:, PAD:].add(ffn.reshape(b, lp - PAD, d))
    return rms_norm(h[:, BLOCK:], norm_final)
```

```python
import contextlib
import numpy as np
import concourse.bass as bass
import concourse.mybir as mybir
from concourse.bass_utils import run_bass_kernel_spmd

F32 = mybir.dt.float32
BF16 = mybir.dt.bfloat16
I32 = mybir.dt.int32
AF = mybir.ActivationFunctionType
ALU = mybir.AluOpType
AX = mybir.AxisListType

D = 2048
NCORE = 8
TOK = 2048
TP = 1024
NPASS = TOK // TP
TT = 128 + TP + 128
NE = 64
CAP = 128
NSLOT = NE * CAP
EPS = 1e-6
IN_W = 6400
Q0, K0, V0, U0, GA0, GP0 = 0, 1024, 1152, 1280, 2304, 4352
DEBUG = False
STOP = 0


class _Stop(Exception):
    pass


class Trk:
    __slots__ = ("w", "r", "multi")

    def __init__(self, multi=False):
        self.w = {}
        self.r = {}
        self.multi = multi


class Eng:
    def __init__(self, name, h, sem, inorder_safe=False):
        self.name = name
        self.h = h
        self.sem = sem
        self.count = 0
        self.known = {}
        self.inorder_safe = inorder_safe
        self.dsems = []
        self.dtot = []
        self.di = 0


class K:
    def __init__(self, nc, stack):
        self.nc = nc
        self.stack = stack
        self.sems = {}
        mk = lambda n: stack.enter_context(nc.semaphore(n))
        self.pe = Eng("pe", nc.tensor, mk("s_pe"), inorder_safe=True)
        self.act = Eng("act", nc.scalar, mk("s_act"))
        self.dve = Eng("dve", nc.vector, mk("s_dve"))
        self.pool = Eng("pool", nc.gpsimd, mk("s_pool"))
        self.sp = Eng("sp", nc.sync, mk("s_sp"))
        self.engs = [self.pe, self.act, self.dve, self.pool, self.sp]
        for e, n in ((self.sp, 20), (self.pool, 20)):
            for i in range(n):
                e.dsems.append(mk(f"d_{e.name}{i}"))
                e.dtot.append(0)
        self.allsems = {}
        for e in self.engs:
            self.allsems[id(e.sem)] = e.sem
            for s in e.dsems:
                self.allsems[id(s)] = s

    def _wait(self, eng, sem, val):
        if val <= 0:
            return
        key = id(sem)
        if eng.known.get(key, 0) >= val:
            return
        eng.h.wait_ge(sem, val)
        eng.known[key] = val

    def _deps(self, eng, reads, writes):
        deps = {}

        def add(d):
            for key, val in d.items():
                if deps.get(key, 0) < val:
                    deps[key] = val

        for t in reads:
            add(t.w)
        for t in writes:
            if not t.multi:
                add(t.w)
            add(t.r)
        for key, val in deps.items():
            sem = self.allsems[key]
            if sem is eng.sem and eng.inorder_safe:
                continue
            self._wait(eng, sem, val)

    def _mark(self, reads, writes, sem, val):
        key = id(sem)
        for t in reads:
            t.r[key] = max(t.r.get(key, 0), val)
        for t in writes:
            if t.multi:
                t.w[key] = max(t.w.get(key, 0), val)
            else:
                t.w = {key: val}
                t.r = {}

    def op(self, eng, reads, writes, fns):
        if not isinstance(fns, (list, tuple)):
            fns = [fns]
        self._deps(eng, reads, writes)
        ins = None
        for f in fns:
            ins = f(eng.h)
        eng.count += 1
        ins.then_inc(eng.sem, 1)
        self._mark(reads, writes, eng.sem, eng.count)

    def dma(self, eng, reads, writes, fn):
        i = eng.di
        eng.di = (i + 1) % len(eng.dsems)
        sem = eng.dsems[i]
        self._wait(eng, sem, eng.dtot[i])
        self._deps(eng, reads, writes)
        ins = fn(eng.h)
        eng.dtot[i] += 16
        ins.then_inc(sem, 16)
        self._mark(reads, writes, sem, eng.dtot[i])

    def barrier(self, pool=True):
        for e in self.engs:
            if e is self.pool and not pool:
                continue
            for o in self.engs:
                if o is not e:
                    self._wait(e, o.sem, o.count)
                for s, tot in zip(o.dsems, o.dtot):
                    self._wait(e, s, tot)

    def finish(self):
        for o in self.engs:
            self._wait(self.sp, o.sem, o.count)
            for s, tot in zip(o.dsems, o.dtot):
                self._wait(self.sp, s, tot)


def build_program():
    nc = bass.Bass("TRN2", target_bir_lowering=False)

    def din(name, shape, dt=F32):
        return nc.dram_tensor(name, list(shape), dt, kind="ExternalInput").ap()

    xin = din("xin", [128 + TOK, D])
    meta = din("meta", [128, D])
    w_in = din("w_in", [D, IN_W])
    pool_mix = din("pool_mix", [4, 256, 256])
    w_ab = din("w_ab", [1024, D])
    w_pb = din("w_pb", [1024, D])
    w_out = din("w_out", [D, D])
    w_rt = din("w_rt", [D, 72])
    ne_decl = 1 if 10 <= STOP <= 29 else NE
    w_gate = din("w_gate", [ne_decl, D, 512])
    w_up = din("w_up", [ne_decl, D, 512])
    w_down = din("w_down", [ne_decl, 512, D])
    gmix_rep = din("gmix_rep", [128, D])
    gffn_rep = din("gffn_rep", [128, D])
    gfin_rep = din("gfin_rep", [128, D])
    pscale = din("pscale", [128, 8])
    sinks_rep = din("sinks_rep", [128, 16])
    brt_rep = din("brt_rep", [128, 72])
    base_rep = din("base_rep", [128, 64])
    ident_in = din("ident_in", [128, 128])
    tri_in = din("tri_in", [128, 128])
    ones_in = din("ones_in", [128, 128])
    bias_pg = din("bias_pg", [128, 2048])
    bias_c = din("bias_c", [128, 2048])
    bias_pf = din("bias_pf", [128, 2048])
    bias_mg = din("bias_mg", [128, 2048])
    bias_mf = din("bias_mf", [128, 2048])
    out = nc.dram_tensor("out", [TOK, D], F32, kind="ExternalOutput").ap()
    sk = "ExternalOutput" if DEBUG else "Internal"
    h_scr = nc.dram_tensor("h_scr", [TOK, D], F32, kind=sk).ap()
    xs_scr = nc.dram_tensor("xs_scr", [NSLOT, D], BF16).ap()
    y_scr = nc.dram_tensor("y_scr", [NSLOT, D], F32).ap()
    if DEBUG:
        dbg_attn = nc.dram_tensor("dbg_attn", [128, 8 * TP], BF16, kind="ExternalOutput").ap()
        dbg_pool = nc.dram_tensor("dbg_pool", [128, 8 * TP], BF16, kind="ExternalOutput").ap()
        dbg_route = nc.dram_tensor("dbg_route", [128, 64], F32, kind="ExternalOutput").ap()

    try:
      with contextlib.ExitStack() as top:
        k = K(nc, top)

        def stop_at(code):
            if STOP == code:
                k.barrier()
                k.finish()
                raise _Stop()

        PE, ACT, DVE, POOL, SP = k.pe, k.act, k.dve, k.pool, k.sp

        uniq = {"n": 0}

        def sb(stack, name, shape, dt):
            uniq["n"] += 1
            return stack.enter_context(nc.sbuf_tensor(f"{name}_{uniq['n']}", list(shape), dt))

        ps = top.enter_context(nc.psum_tensor("ps", [128, 6, 512], F32))
        psb = top.enter_context(nc.psum_tensor("psb", [128, 2, 1024], BF16))
        PS = [Trk() for _ in range(6)]
        PSB = [Trk() for _ in range(2)]
        ident_f = sb(top, "ident_f", [128, 128], F32)
        ident_b = sb(top, "ident_b", [128, 128], BF16)
        tri_f = sb(top, "tri_f", [128, 128], F32)
        ones_f = sb(top, "ones_f", [128, 128], F32)
        t_const = Trk()
        slot_i = sb(top, "slot_i", [128, 32], I32)
        wts = sb(top, "wts", [128, 32], F32)
        t_route = Trk(multi=True)
        zero_b = sb(top, "zero_b", [128, D], BF16)
        t_zero = Trk()
        T_HS = Trk(multi=True)
        T_XS = Trk(multi=True)
        T_YS = Trk(multi=True)
        T_OUT = Trk(multi=True)

        k.dma(SP, [], [t_const], lambda h: h.dma_start(out=ident_f[:], in_=ident_in))
        k.dma(SP, [], [t_const], lambda h: h.dma_start(out=tri_f[:], in_=tri_in))
        k.dma(SP, [], [t_const], lambda h: h.dma_start(out=ones_f[:], in_=ones_in))
        k.dma(POOL, [], [t_const], lambda h: h.dma_start(out=ident_b[:], in_=ident_in))
        k.op(DVE, [], [t_zero], lambda h: h.memset(zero_b[:], 0.0))
        def zero_fill():
            for i in range(NSLOT // 128):
                k.dma(SP, [t_zero], [T_XS], lambda h, i=i: h.dma_start(out=xs_scr[i * 128:(i + 1) * 128, :], in_=zero_b[:]))

        stop_at(10)
        rr = {"ev": 0}

        def evac(reads, writes, out_ap, in_ap, scale=None):
            rr["ev"] += 1
            if rr["ev"] % 2 == 0:
                if scale is None:
                    k.op(ACT, reads, writes, lambda h: h.activation(out=out_ap, in_=in_ap, func=AF.Copy))
                else:
                    k.op(ACT, reads, writes, lambda h: h.activation(out=out_ap, in_=in_ap, func=AF.Copy, scale=scale))
            else:
                if scale is None:
                    k.op(DVE, reads, writes, lambda h: h.tensor_copy(out=out_ap, in_=in_ap))
                else:
                    k.op(DVE, reads, writes, lambda h: h.tensor_scalar(out=out_ap, in0=in_ap, scalar1=scale, scalar2=None, op0=ALU.mult))

        def rstd_of(stack_tiles, src_ap, nparts, junk, ss, rs, reads, t_junk, t_ss):
            k.op(ACT, reads, [t_junk, t_ss], lambda h: h.activation(out=junk[0:nparts, :], in_=src_ap, func=AF.Square, accum_out=ss[0:nparts, 0:1]))
            k.op(ACT, [t_ss], [t_ss], lambda h: h.activation(out=ss[0:nparts, 1:2], in_=ss[0:nparts, 0:1], func=AF.Sqrt, bias=EPS, scale=1.0 / D))
            k.op(DVE, [t_ss], [t_ss], lambda h: h.reciprocal(out=rs[0:nparts, 0:1], in_=ss[0:nparts, 1:2]))

        with contextlib.ExitStack() as mx:
            hnT = sb(mx, "hnT", [128, 16, TT], BF16)
            T_hn = [Trk() for _ in range(10)]
            attnT = sb(mx, "attnT", [128, 8, TP], BF16)
            T_attn = Trk(multi=True)
            poolT = sb(mx, "poolT", [128, 8, TP], BF16)
            T_pool = Trk(multi=True)
            T_mrg = Trk(multi=True)
            wsl = [sb(mx, f"wsl{i}", [128, 16, 512], BF16) for i in range(4)]
            T_w = [Trk() for _ in range(4)]
            wi = {"i": 0}
            psc = sb(mx, "psc", [128, 8], F32)
            esink = sb(mx, "esink", [128, 16], F32)
            wpm = sb(mx, "wpm", [128, 8, 256], BF16)
            t_mc = Trk()
            k.dma(SP, [], [t_mc], lambda h: h.dma_start(out=psc[:], in_=pscale))
            k.dma(SP, [], [t_mc], lambda h: h.dma_start(out=esink[:], in_=sinks_rep))
            k.op(ACT, [t_mc], [t_mc], lambda h: h.activation(out=esink[:], in_=esink[:], func=AF.Exp))
            for g in range(4):
                k.dma(POOL, [], [t_mc], lambda h, g=g: h.dma_start(
                    out=wpm[:, 2 * g:2 * g + 2, :], in_=pool_mix[g].rearrange("(kc p) d -> p kc d", p=128)))

            def load_w(src_ap, nk, ncols, col_off=0):
                i = wi["i"]
                wi["i"] = (i + 1) % 4
                k.dma(POOL, [], [T_w[i]], lambda h: h.dma_start(
                    out=wsl[i][:, 0:nk, col_off:col_off + ncols], in_=src_ap.rearrange("(kc p) m -> p kc m", p=128)))
                return wsl[i], T_w[i]

            for ps_i in range(NPASS):
                r0 = ps_i * TP
                with contextlib.ExitStack() as st:
                    gmix = sb(st, "gmix", [128, D], F32)
                    t_gm = Trk()
                    k.dma(SP, [], [t_gm], lambda h: h.dma_start(out=gmix[:], in_=gmix_rep))
                    xt = [sb(st, f"xt{i}", [128, D], F32) for i in range(2)]
                    T_xt = [Trk() for _ in range(2)]
                    xn = [sb(st, f"xn{i}", [128, D], BF16) for i in range(2)]
                    T_xn = [Trk() for _ in range(2)]
                    junk = sb(st, "junk", [128, D], BF16)
                    t_junk = Trk()
                    ssb = [sb(st, f"ssb{i}", [128, 4], F32) for i in range(2)]
                    T_ss = [Trk() for _ in range(2)]
                    for tb in range(10):
                        i = tb % 2
                        np_ = 128
                        src = meta if tb == 9 else xin[r0 + tb * 128: r0 + (tb + 1) * 128, :]
                        c0 = 1152 if tb == 9 else tb * 128
                        k.dma(SP, [], [T_xt[i]], lambda h, i=i, src=src, np_=np_: h.dma_start(out=xt[i][0:np_, :], in_=src))
                        rstd_of(None, xt[i][0:np_, :], np_, junk, ssb[i], ssb[i][:, 2:3], [T_xt[i]], t_junk, T_ss[i])
                        k.op(DVE, [T_xt[i], T_ss[i], t_gm], [T_xn[i]], lambda h, i=i, np_=np_: h.scalar_tensor_tensor(
                            out=xn[i][0:np_, :], in0=xt[i][0:np_, :], scalar=ssb[i][0:np_, 2:3], in1=gmix[0:np_, :],
                            op0=ALU.mult, op1=ALU.mult))
                        for half in range(2):
                            k.op(PE, [T_xn[i], t_const], [PSB[half]], [
                                (lambda h, i=i, c=c, half=half, np_=np_: h.transpose(
                                    out=psb[:, half, (c % 8) * 128:(c % 8) * 128 + np_],
                                    in_=xn[i][0:np_, c * 128:(c + 1) * 128], identity=ident_b[0:np_, 0:np_]))
                                for c in range(half * 8, half * 8 + 8)])
                            src_ps = psb[:, half, :].rearrange("p (c t) -> p c t", t=128)[:, :, 0:np_]
                            evac([PSB[half]], [T_hn[tb]], hnT[:, half * 8:half * 8 + 8, c0:c0 + np_], src_ps)

                    k.barrier()
                    if ps_i == 0:
                        zero_fill()
                    stop_at(11)

                with contextlib.ExitStack() as st:
                    qT = sb(st, "qT", [128, 8, TP], BF16)
                    T_q = Trk(multi=True)
                    bpg = sb(st, "bpg", [128, 2048], BF16)
                    bcc = sb(st, "bcc", [128, 2048], BF16)
                    bmg = sb(st, "bmg", [128, 2048], BF16)
                    t_bt = Trk()
                    k.dma(POOL, [], [t_bt], lambda h: h.dma_start(out=bpg[:], in_=(bias_pf if ps_i == 0 else bias_pg)))
                    k.dma(POOL, [], [t_bt], lambda h: h.dma_start(out=bcc[:], in_=bias_c))
                    k.dma(POOL, [], [t_bt], lambda h: h.dma_start(out=bmg[:], in_=(bias_mf if ps_i == 0 else bias_mg)))
                    kT2 = sb(st, "kT2", [128, 4, TT], BF16)
                    T_k = Trk(multi=True)
                    vtm = sb(st, "vtm", [128, 10, 2, 65], BF16)
                    T_v = Trk(multi=True)
                    k.op(DVE, [], [T_v], lambda h: h.memset(vtm[:], 1.0))
                    bank = {"i": 0}

                    def nb():
                        bank["i"] = (bank["i"] + 1) % 6
                        return bank["i"]

                    def inproj_fm(wt, tw, nk, mcol, nsl, rhs_of, rhs_trk, evac_fn):
                        for (n0, n1) in nsl:
                            b = nb()
                            k.op(PE, [tw] + rhs_trk, [PS[b]], [
                                (lambda h, kc=kc, b=b, n0=n0, n1=n1: h.matmul(
                                    ps[:, b, 0:n1 - n0], lhsT=wt[:, kc, mcol:mcol + 128], rhs=rhs_of(kc, n0, n1),
                                    start=(kc == 0), stop=(kc == nk - 1)))
                                for kc in range(nk)])
                            evac_fn(b, n0, n1)

                    hn_rhs = lambda kc, n0, n1: hnT[:, kc, n0:n1]
                    NS_ALL = [(0, 512), (512, 1024), (1024, TT)]
                    NS_MAIN = [(128, 640), (640, 1152)]
                    wt, tw = None, None
                    i = wi["i"]
                    wi["i"] = (i + 1) % 4
                    wt, tw = wsl[i], T_w[i]
                    k.op(DVE, [], [tw], lambda h: h.memset(wt[:], 0.0))
                    for g in range(2):
                        for hf in range(2):
                            m = g * 2 + hf
                            k.dma(POOL, [], [tw], lambda h, g=g, hf=hf, m=m: h.dma_start(
                                out=wt[:, :, m * 128 + hf * 64: m * 128 + hf * 64 + 64],
                                in_=w_in[:, K0 + g * 64: K0 + g * 64 + 64].rearrange("(kc p) m -> p kc m", p=128)))
                    for m in range(4):
                        inproj_fm(wt, tw, 16, m * 128, NS_ALL, hn_rhs, T_hn,
                                  lambda b, n0, n1, m=m: evac([PS[b]], [T_k], kT2[:, m, n0:n1], ps[:, b, 0:n1 - n0]))
                    wv, twv = load_w(w_in[:, V0:V0 + 128], 16, 128)
                    for tb in range(10):
                        np_ = 128
                        c0 = 1152 if tb == 9 else tb * 128
                        b = nb()
                        k.op(PE, [twv, T_hn[tb]], [PS[b]], [
                            (lambda h, kc=kc, b=b, c0=c0, np_=np_: h.matmul(
                                ps[0:np_, b, 0:128], lhsT=hnT[:, kc, c0:c0 + np_], rhs=wv[:, kc, 0:128],
                                start=(kc == 0), stop=(kc == 15)))
                            for kc in range(16)])
                        evac([PS[b]], [T_v], vtm[0:np_, tb, :, 0:64],
                             ps[0:np_, b, 0:128].rearrange("p (g d) -> p g d", g=2))
                    for qh in range(2):
                        wt, tw = load_w(w_in[:, Q0 + qh * 512: Q0 + (qh + 1) * 512], 16, 512)
                        for m in range(4):
                            c = qh * 4 + m
                            inproj_fm(wt, tw, 16, m * 128, NS_MAIN, hn_rhs, T_hn,
                                      lambda b, n0, n1, c=c: evac([PS[b]], [T_q], qT[:, c, n0 - 128:n1 - 128],
                                                                  ps[:, b, 0:n1 - n0], scale=0.125))

                    stop_at(12)
                    pts = [[sb(st, f"pt{r}_{s}", [128, 512], BF16) for s in range(3)] for r in range(2)]
                    T_pt = [[Trk() for s in range(3)] for r in range(2)]
                    scs = [sb(st, f"sc{r}", [128, 512], F32) for r in range(2)]
                    T_sc = [Trk() for r in range(2)]
                    atm = sb(st, "atm", [128, 16, 64], BF16)
                    T_atm = Trk(multi=True)
                    den = sb(st, "den", [128, 4, 8], F32)
                    T_den = [Trk() for _ in range(4)]
                    sbank = {"i": 0}
                    for b in range(TP // 128):
                        if ps_i == 0 and b == 1:
                            k.dma(POOL, [], [t_bt], lambda h: h.dma_start(out=bpg[:], in_=bias_pg))
                            k.dma(POOL, [], [t_bt], lambda h: h.dma_start(out=bmg[:], in_=bias_mg))
                        btabs = [bpg, bcc, bmg]
                        kcols = [b * 128, (b + 1) * 128, 1152]
                        vblk = [b, b + 1, 9]
                        for hq in range(4):
                            g = hq // 2
                            r = hq % 2
                            for s in range(3):
                                sbk = sbank["i"]
                                sbank["i"] = (sbank["i"] + 1) % 2
                                fns = []
                                for i in range(4):
                                    hd = hq * 4 + i
                                    c, hf = hd // 2, hd % 2
                                    fns.append(lambda h, s=s, sbk=sbk, i=i, c=c, hf=hf, g=g, b=b: h.matmul(
                                        ps[:, sbk, i * 128:(i + 1) * 128],
                                        lhsT=kT2[:, g * 2 + hf, kcols[s]:kcols[s] + 128],
                                        rhs=qT[:, c, b * 128:(b + 1) * 128],
                                        start=True, stop=True))
                                k.op(PE, [T_k, T_q], [PS[sbk]], fns)
                                k.op(DVE, [PS[sbk], t_bt], [T_sc[sbk]], lambda h, s=s, sbk=sbk, hq=hq: h.tensor_tensor(
                                    out=scs[sbk][:], in0=ps[:, sbk, :], in1=btabs[s][:, hq * 512:(hq + 1) * 512], op=ALU.add))
                                k.op(ACT, [T_sc[sbk]], [T_pt[r][s]], lambda h, r=r, s=s, sbk=sbk: h.activation(
                                    out=pts[r][s][:], in_=scs[sbk][:], func=AF.Exp))
                            if b == 0 and hq == 0:
                                stop_at(20)
                            ob = 2 + hq
                            fns = []
                            for i in range(4):
                                for s in range(3):
                                    fns.append(lambda h, i=i, s=s, r=r, g=g, ob=ob: h.matmul(
                                        ps[:, ob, i * 65:(i + 1) * 65], lhsT=pts[r][s][:, i * 128:(i + 1) * 128],
                                        rhs=vtm[:, vblk[s], g, :], start=(s == 0), stop=(s == 2)))
                            k.op(PE, [T_pt[r][0], T_pt[r][1], T_pt[r][2], T_v], [PS[ob]], fns)
                            if b == 0 and hq == 0:
                                stop_at(21)
                            ov = ps[:, ob, 0:260].rearrange("p (i d) -> p i d", d=65)
                            k.op(DVE, [PS[ob], t_mc], [T_den[hq]], lambda h, hq=hq, ov=ov: h.tensor_tensor(
                                out=den[:, hq, 0:4], in0=ov[:, :, 64], in1=esink[:, hq * 4:hq * 4 + 4], op=ALU.add))
                            k.op(DVE, [T_den[hq]], [T_den[hq]], lambda h, hq=hq: h.reciprocal(out=den[:, hq, 4:8], in_=den[:, hq, 0:4]))
                            for i in range(4):
                                k.op(DVE, [PS[ob], T_den[hq]], [T_atm], lambda h, hq=hq, i=i, ov=ov: h.tensor_scalar(
                                    out=atm[:, hq * 4 + i, :], in0=ov[:, i, 0:64], scalar1=den[:, hq, 4 + i:5 + i], scalar2=None,
                                    op0=ALU.mult))
                        if b == 0:
                            stop_at(22)
                        k.op(PE, [T_atm, t_const], [PSB[0]], [
                            (lambda h, c=c: h.transpose(out=psb[:, 0, c * 128:(c + 1) * 128],
                                                        in_=atm[:, 2 * c:2 * c + 2, :].rearrange("p a d -> p (a d)"),
                                                        identity=ident_b[:, :]))
                            for c in range(8)])
                        k.op(DVE, [PSB[0]], [T_attn], lambda h, b=b: h.tensor_copy(
                            out=attnT[:, :, b * 128:(b + 1) * 128], in_=psb[:, 0, :].rearrange("p (c t) -> p c t", t=128)))
                        T_atm.w = {}
                        if b == 0:
                            stop_at(23)
                    if DEBUG and ps_i == 0:
                        k.dma(SP, [T_attn], [T_OUT], lambda h: h.dma_start(out=dbg_attn, in_=attnT[:].rearrange("p c t -> p (c t)")))
                    k.barrier()

                stop_at(13)
                with contextlib.ExitStack() as st:
                    ut = [sb(st, f"ut{i}", [128, 1152], F32) for i in range(2)]
                    T_u = [Trk(multi=True) for _ in range(2)]
                    ta = sb(st, "pa", [128, 1152], F32)
                    tb_ = sb(st, "pb", [128, 1152], F32)
                    T_ta, T_tb = Trk(), Trk()
                    mixT = sb(st, "mixT", [128, 8, TP], BF16)
                    T_mix = [Trk() for _ in range(8)]
                    NS_U = [(0, 512), (512, 1024), (1024, 1152)]
                    WIN = [2, 2, 4, 4, 8, 8, 16, 16]
                    bank["i"] = 0
                    for uh in range(2):
                        wt, tw = load_w(w_in[:, U0 + uh * 512: U0 + (uh + 1) * 512], 16, 512)
                        for m in range(4):
                            c = uh * 4 + m
                            ui = c % 2
                            T_u[ui].w = {}
                            inproj_fm(wt, tw, 16, m * 128, NS_U, hn_rhs, T_hn,
                                      lambda b, n0, n1, ui=ui: evac([PS[b]], [T_u[ui]], ut[ui][:, n0:n1], ps[:, b, 0:n1 - n0]))
                            u = ut[ui]
                            w = WIN[c]
                            k.op(DVE, [T_u[ui]], [T_ta], lambda h, u=u: h.tensor_tensor(
                                out=ta[:, 1:1152], in0=u[:, 1:1152], in1=u[:, 0:1151], op=ALU.add))
                            cur, tcur = ta, T_ta
                            if w >= 4:
                                k.op(DVE, [T_ta], [T_tb], lambda h: h.tensor_tensor(
                                    out=tb_[:, 3:1152], in0=ta[:, 3:1152], in1=ta[:, 1:1150], op=ALU.add))
                                cur, tcur = tb_, T_tb
                            if w >= 8:
                                k.op(DVE, [T_tb], [T_ta], lambda h: h.tensor_tensor(
                                    out=ta[:, 7:1152], in0=tb_[:, 7:1152], in1=tb_[:, 3:1148], op=ALU.add))
                                cur, tcur = ta, T_ta
                            if w >= 16:
                                k.op(DVE, [T_ta], [T_tb], lambda h: h.tensor_tensor(
                                    out=tb_[:, 15:1152], in0=ta[:, 15:1152], in1=ta[:, 7:1144], op=ALU.add))
                                cur, tcur = tb_, T_tb
                            k.op(DVE, [tcur, T_u[ui]], [T_mix[c]], lambda h, cur=cur, u=u, w=w, c=c: h.scalar_tensor_tensor(
                                out=mixT[:, c, :], in0=cur[:, 128:1152], scalar=1.0 / w, in1=u[:, 128:1152],
                                op0=ALU.mult, op1=ALU.subtract))
                            if c % 2 == 1:
                                g = c // 2
                                for dc in range(2):
                                    for (n0, n1) in [(0, 512), (512, 1024)]:
                                        b = nb()
                                        k.op(PE, [T_mix[c - 1], T_mix[c], t_mc], [PS[b]], [
                                            (lambda h, kc=kc, b=b, g=g, dc=dc, n0=n0, n1=n1: h.matmul(
                                                ps[:, b, 0:512], lhsT=wpm[:, 2 * g + kc, dc * 128:(dc + 1) * 128],
                                                rhs=mixT[:, 2 * g + kc, n0:n1], start=(kc == 0), stop=(kc == 1)))
                                            for kc in range(2)])
                                        evac([PS[b], t_mc], [T_pool], poolT[:, 2 * g + dc, n0:n1], ps[:, b, 0:512],
                                             scale=psc[:, 2 * g + dc:2 * g + dc + 1])
                    if DEBUG and ps_i == 0:
                        k.dma(SP, [T_pool], [T_OUT], lambda h: h.dma_start(out=dbg_pool, in_=poolT[:].rearrange("p c t -> p (c t)")))
                    k.barrier()

                stop_at(14)
                with contextlib.ExitStack() as st:
                    mergedT = sb(st, "mergedT", [128, 16, TP], BF16)
                    sga = [sb(st, f"sga{i}", [128, 512], F32) for i in range(2)]
                    T_sg = [Trk() for _ in range(2)]
                    tmpa = sb(st, "tmpa", [128, 512], F32)
                    tmpp = sb(st, "tmpp", [128, 512], F32)
                    T_tma, T_tmp = Trk(), Trk()
                    at_rhs = lambda kc, n0, n1: attnT[:, kc, n0 - 128:n1 - 128]
                    pl_rhs = lambda kc, n0, n1: poolT[:, kc, n0 - 128:n1 - 128]
                    bank["i"] = 0
                    T_mrg.w = {}
                    for mg in range(4):
                        wga, tga = load_w(w_in[:, GA0 + mg * 512: GA0 + (mg + 1) * 512], 16, 512)
                        wgp, tgp = load_w(w_in[:, GP0 + mg * 512: GP0 + (mg + 1) * 512], 16, 512)
                        i = wi["i"]
                        wi["i"] = (i + 1) % 4
                        wbr, tbr = wsl[i], T_w[i]
                        k.dma(POOL, [], [tbr], lambda h, mg=mg: h.dma_start(
                            out=wbr[:, 0:8, :], in_=w_ab[:, mg * 512:(mg + 1) * 512].rearrange("(kc p) m -> p kc m", p=128)))
                        k.dma(POOL, [], [tbr], lambda h, mg=mg: h.dma_start(
                            out=wbr[:, 8:16, :], in_=w_pb[:, mg * 512:(mg + 1) * 512].rearrange("(kc p) m -> p kc m", p=128)))
                        wbr_p = wbr[:, 8:16, :]
                        for m in range(4):
                            mc = mg * 4 + m
                            for (n0, n1) in NS_MAIN:
                                def ev_sig(b, n0_, n1_, j):
                                    k.op(ACT, [PS[b]], [T_sg[j]], lambda h: h.activation(out=sga[j][:], in_=ps[:, b, :], func=AF.Sigmoid))
                                inproj_fm(wga, tga, 16, m * 128, [(n0, n1)], hn_rhs, T_hn, lambda b, a, c_: ev_sig(b, a, c_, 0))
                                inproj_fm(wbr, tbr, 8, m * 128, [(n0, n1)], at_rhs, [T_attn],
                                          lambda b, a, c_: k.op(DVE, [PS[b], T_sg[0]], [T_tma], lambda h: h.tensor_tensor(
                                              out=tmpa[:], in0=ps[:, b, :], in1=sga[0][:], op=ALU.mult)))
                                inproj_fm(wgp, tgp, 16, m * 128, [(n0, n1)], hn_rhs, T_hn, lambda b, a, c_: ev_sig(b, a, c_, 1))
                                inproj_fm(wbr_p, tbr, 8, m * 128, [(n0, n1)], pl_rhs, [T_pool],
                                          lambda b, a, c_: k.op(DVE, [PS[b], T_sg[1]], [T_tmp], lambda h: h.tensor_tensor(
                                              out=tmpp[:], in0=ps[:, b, :], in1=sga[1][:], op=ALU.mult)))
                                k.op(DVE, [T_tma, T_tmp], [T_mrg], lambda h, mc=mc, n0=n0, n1=n1: h.tensor_tensor(
                                    out=mergedT[:, mc, n0 - 128:n1 - 128], in0=tmpa[:], in1=tmpp[:], op=ALU.add))

                    stop_at(15)
                    xr = [sb(st, f"xr{i}", [128, 512], F32) for i in range(2)]
                    T_xr = [Trk() for _ in range(2)]
                    ho = [sb(st, f"ho{i}", [128, 512], F32) for i in range(2)]
                    T_ho = [Trk() for _ in range(2)]
                    bank["i"] = 0
                    cnt = 0
                    for ds in range(4):
                        wt, tw = load_w(w_out[:, ds * 512:(ds + 1) * 512], 16, 512)
                        for tb in range(TP // 128):
                            i = cnt % 2
                            cnt += 1
                            row = ps_i * TP + tb * 128
                            k.dma(SP, [], [T_xr[i]], lambda h, i=i, row=row, ds=ds: h.dma_start(
                                out=xr[i][:], in_=xin[128 + row:128 + row + 128, ds * 512:(ds + 1) * 512]))
                            b = nb()
                            k.op(PE, [tw, T_mrg], [PS[b]], [
                                (lambda h, kc=kc, b=b, tb=tb: h.matmul(
                                    ps[:, b, :], lhsT=mergedT[:, kc, tb * 128:(tb + 1) * 128], rhs=wt[:, kc, :],
                                    start=(kc == 0), stop=(kc == 15)))
                                for kc in range(16)])
                            k.op(DVE, [PS[b], T_xr[i]], [T_ho[i]], lambda h, i=i, b=b: h.tensor_tensor(
                                out=ho[i][:], in0=ps[:, b, :], in1=xr[i][:], op=ALU.add))
                            k.dma(SP, [T_ho[i]], [T_HS], lambda h, i=i, row=row, ds=ds: h.dma_start(
                                out=h_scr[row:row + 128, ds * 512:(ds + 1) * 512], in_=ho[i][:]))
                    k.barrier()
                T_attn.w = {}
                T_pool.w = {}
            k.barrier()
        k.barrier()
        if STOP == 1:
            k.finish()
            return nc

        with contextlib.ExitStack() as moe:
            ew = [[sb(moe, f"ew{r}_{j}", [128, 16, 512], BF16) for j in range(3)] for r in range(2)]
            T_ew = [[Trk() for j in range(3)] for r in range(2)]

            def load_expert(e):
                r = e % 2
                k.dma(POOL, [], [T_ew[r][0]], lambda h: h.dma_start(
                    out=ew[r][0][:], in_=w_gate[e].rearrange("(p kc) f -> p kc f", kc=16)))
                k.dma(POOL, [], [T_ew[r][1]], lambda h: h.dma_start(
                    out=ew[r][1][:], in_=w_up[e].rearrange("(p kc) f -> p kc f", kc=16)))
                k.dma(POOL, [], [T_ew[r][2]], lambda h: h.dma_start(
                    out=ew[r][2][:].rearrange("p (fc a) f -> p fc (a f)", a=4), in_=w_down[e].rearrange("(fc p) d -> p fc d", p=128)))

            load_expert(0)
            load_expert(1)

            with contextlib.ExitStack() as st:
                gffn = sb(st, "gffn", [128, D], F32)
                wr = sb(st, "wr", [128, 16, 72], F32)
                brt = sb(st, "brt", [128, 72], F32)
                basef = sb(st, "basef", [128, 64], F32)
                carry = sb(st, "carry", [128, 64], F32)
                t_rc = Trk()
                T_carry = Trk()
                k.dma(SP, [], [t_rc], lambda h: h.dma_start(out=gffn[:], in_=gffn_rep))
                k.dma(SP, [], [t_rc], lambda h: h.dma_start(out=wr[:], in_=w_rt.rearrange("(kc p) m -> p kc m", p=128)))
                k.dma(SP, [], [t_rc], lambda h: h.dma_start(out=brt[:], in_=brt_rep))
                k.dma(SP, [], [t_rc], lambda h: h.dma_start(out=basef[:], in_=base_rep))
                k.op(DVE, [], [T_carry], lambda h: h.memset(carry[:], 0.0))
                hh = [sb(st, f"hh{i}", [128, D], F32) for i in range(2)]
                T_hh = [Trk() for _ in range(2)]
                hn2 = [sb(st, f"hn2{i}", [128, D], F32) for i in range(2)]
                T_hn2 = [Trk() for _ in range(2)]
                hnb = [sb(st, f"hnb{i}", [128, D], BF16) for i in range(2)]
                T_hnb = [Trk() for _ in range(2)]
                hT = sb(st, "hT", [128, 16, 128], F32)
                T_hT = Trk(multi=True)
                junk = sb(st, "junk2", [128, D], BF16)
                t_junk = Trk()
                ssb = [sb(st, f"ssr{i}", [128, 4], F32) for i in range(2)]
                T_ss = [Trk() for _ in range(2)]
                sm = sb(st, "sm", [128, 512], F32)
                T_sm = Trk()
                for tb in range(16):
                    i = tb % 2
                    k.dma(SP, [T_HS], [T_hh[i]], lambda h, i=i, tb=tb: h.dma_start(out=hh[i][:], in_=h_scr[tb * 128:(tb + 1) * 128, :]))
                    rstd_of(None, hh[i][:], 128, junk, ssb[i], ssb[i][:, 2:3], [T_hh[i]], t_junk, T_ss[i])
                    k.op(DVE, [T_hh[i], T_ss[i], t_rc], [T_hn2[i]], lambda h, i=i: h.scalar_tensor_tensor(
                        out=hn2[i][:], in0=hh[i][:], scalar=ssb[i][:, 2:3], in1=gffn[:], op0=ALU.mult, op1=ALU.mult))
                    k.op(ACT, [T_hn2[i]], [T_hnb[i]], lambda h, i=i: h.activation(
                        out=hnb[i][:].rearrange("s (kc p) -> s kc p", p=128),
                        in_=hn2[i][:].rearrange("s (p kc) -> s kc p", kc=16), func=AF.Copy))
                    T_hT.w = {}
                    for q4 in range(4):
                        k.op(PE, [T_hn2[i], t_const], [PS[q4]], [
                            (lambda h, i=i, c=c, q4=q4: h.transpose(out=ps[:, q4, (c % 4) * 128:(c % 4 + 1) * 128],
                                                                   in_=hn2[i][:, c * 128:(c + 1) * 128], identity=ident_f[:, :]))
                            for c in range(q4 * 4, q4 * 4 + 4)])
                        evac([PS[q4]], [T_hT], hT[:, q4 * 4:q4 * 4 + 4, :], ps[:, q4, :].rearrange("p (c t) -> p c t", t=128))
                    k.op(PE, [T_hT, t_rc], [PS[4]], [
                        (lambda h, c=c: h.matmul(ps[:, 4, 0:72], lhsT=hT[:, c, :], rhs=wr[:, c, :], start=(c == 0), stop=(c == 15)))
                        for c in range(16)])
                    LG, OHG, ESEL, OH1, ES2, OH2, E1, E2, AA, POS, TMP = 0, 72, 80, 88, 96, 104, 112, 176, 240, 304, 368
                    GM, NGM, SUMG, PG, M1, M2, D21, W1c, W2c, S1, S2 = 440, 441, 442, 443, 444, 445, 446, 447, 448, 449, 450
                    EG = 456
                    c1 = lambda a: sm[:, a:a + 1]
                    c8 = lambda a: sm[:, a:a + 8]
                    c64 = lambda a: sm[:, a:a + 64]
                    R = [T_sm]
                    W = [T_sm]
                    k.op(DVE, [PS[4], t_rc, T_sm], W, lambda h: h.tensor_tensor(out=sm[:, LG:LG + 72], in0=ps[:, 4, 0:72], in1=brt[:], op=ALU.add))
                    k.op(DVE, R, W, lambda h: h.tensor_reduce(out=c1(GM), in_=c8(LG), axis=AX.X, op=ALU.max))
                    k.op(DVE, R, W, lambda h: h.tensor_scalar(out=c8(OHG), in0=c8(LG), scalar1=c1(GM), scalar2=None, op0=ALU.is_equal))
                    k.op(DVE, R, W, lambda h: h.tensor_scalar(out=c1(NGM), in0=c1(GM), scalar1=-1.0, scalar2=None, op0=ALU.mult))
                    k.op(ACT, R, W, lambda h: h.activation(out=c8(EG), in_=c8(LG), func=AF.Exp, bias=c1(NGM), accum_out=c1(SUMG)))
                    k.op(DVE, R, W, lambda h: h.reciprocal(out=c1(PG), in_=c1(SUMG)))
                    k.op(DVE, R, W, lambda h: h.tensor_scalar(out=c8(ESEL), in0=sm[:, LG + 8:LG + 16], scalar1=c1(OHG), scalar2=None, op0=ALU.mult))
                    for g in range(1, 8):
                        k.op(DVE, R, W, lambda h, g=g: h.scalar_tensor_tensor(
                            out=c8(ESEL), in0=sm[:, LG + 8 + g * 8:LG + 16 + g * 8], scalar=c1(OHG + g), in1=c8(ESEL),
                            op0=ALU.mult, op1=ALU.add))
                    k.op(DVE, R, W, lambda h: h.tensor_reduce(out=c1(M1), in_=c8(ESEL), axis=AX.X, op=ALU.max))
                    k.op(DVE, R, W, lambda h: h.tensor_scalar(out=c8(OH1), in0=c8(ESEL), scalar1=c1(M1), scalar2=None, op0=ALU.is_equal))
                    k.op(DVE, R, W, lambda h: h.scalar_tensor_tensor(out=c8(ES2), in0=c8(OH1), scalar=-1.0e30, in1=c8(ESEL),
                                                                   op0=ALU.mult, op1=ALU.add))
                    k.op(DVE, R, W, lambda h: h.tensor_reduce(out=c1(M2), in_=c8(ES2), axis=AX.X, op=ALU.max))
                    k.op(DVE, R, W, lambda h: h.tensor_scalar(out=c8(OH2), in0=c8(ES2), scalar1=c1(M2), scalar2=None, op0=ALU.is_equal))
                    k.op(DVE, R, W, lambda h: h.tensor_tensor(out=c1(D21), in0=c1(M2), in1=c1(M1), op=ALU.subtract))
                    k.op(ACT, R, W, lambda h: h.activation(out=c1(D21), in_=c1(D21), func=AF.Exp))
                    k.op(DVE, R, W, lambda h: h.tensor_scalar(out=c1(D21), in0=c1(D21), scalar1=1.0, scalar2=None, op0=ALU.add))
                    k.op(DVE, R, W, lambda h: h.reciprocal(out=c1(D21), in_=c1(D21)))
                    k.op(DVE, R + [t_route], W + [t_route], lambda h, tb=tb: h.tensor_tensor(out=wts[:, tb:tb + 1], in0=c1(PG), in1=c1(D21), op=ALU.mult))
                    k.op(DVE, R + [t_route], W + [t_route], lambda h, tb=tb: h.tensor_tensor(out=wts[:, 16 + tb:17 + tb], in0=c1(PG), in1=wts[:, tb:tb + 1], op=ALU.subtract))
                    for g in range(8):
                        k.op(DVE, R, W, lambda h, g=g: h.tensor_scalar(out=sm[:, E1 + g * 8:E1 + g * 8 + 8], in0=c8(OH1), scalar1=c1(OHG + g), scalar2=None, op0=ALU.mult))
                        k.op(DVE, R, W, lambda h, g=g: h.tensor_scalar(out=sm[:, E2 + g * 8:E2 + g * 8 + 8], in0=c8(OH2), scalar1=c1(OHG + g), scalar2=None, op0=ALU.mult))
                    k.op(DVE, R, W, lambda h: h.tensor_tensor(out=c64(AA), in0=c64(E1), in1=c64(E2), op=ALU.add))
                    k.op(PE, [T_sm, t_const], [PS[5]], [
                        lambda h: h.matmul(ps[:, 5, 0:64], lhsT=tri_f[:, :], rhs=c64(AA), start=True, stop=True),
                        lambda h: h.matmul(ps[:, 5, 64:128], lhsT=ones_f[:, :], rhs=c64(AA), start=True, stop=True)])
                    k.op(DVE, [PS[5], T_carry, T_sm], W, lambda h: h.tensor_tensor(out=c64(POS), in0=ps[:, 5, 0:64], in1=carry[:], op=ALU.add))
                    k.op(DVE, [PS[5], T_carry], [T_carry], lambda h: h.tensor_tensor(out=carry[:], in0=ps[:, 5, 64:128], in1=carry[:], op=ALU.add))
                    k.op(DVE, R + [t_rc], W, lambda h: h.scalar_tensor_tensor(out=c64(POS), in0=c64(POS), scalar=float(CAP - 1), in1=basef[:],
                                                                            op0=ALU.min, op1=ALU.add))
                    k.op(DVE, R, W, lambda h: h.tensor_tensor(out=c64(TMP), in0=c64(POS), in1=c64(E1), op=ALU.mult))
                    k.op(DVE, R, W, lambda h: h.tensor_reduce(out=c1(S1), in_=c64(TMP), axis=AX.X, op=ALU.add))
                    k.op(DVE, R, W, lambda h: h.tensor_tensor(out=c64(TMP), in0=c64(POS), in1=c64(E2), op=ALU.mult))
                    k.op(DVE, R, W, lambda h: h.tensor_reduce(out=c1(S2), in_=c64(TMP), axis=AX.X, op=ALU.add))
                    k.op(DVE, R + [t_route], W + [t_route], lambda h, tb=tb: h.tensor_copy(out=slot_i[:, tb:tb + 1], in_=c1(S1)))
                    k.op(DVE, R + [t_route], W + [t_route], lambda h, tb=tb: h.tensor_copy(out=slot_i[:, 16 + tb:17 + tb], in_=c1(S2)))
                    for j in range(2):
                        k.dma(POOL, [T_hnb[i], t_route], [T_XS], lambda h, i=i, tb=tb, j=j: h.indirect_dma_start(
                            out=xs_scr[:, :], out_offset=bass.IndirectOffsetOnAxis(ap=slot_i[:, 16 * j + tb:16 * j + tb + 1], axis=0),
                            in_=hnb[i][:, :], in_offset=None))
                if DEBUG:
                    dr = sb(st, "dr", [128, 64], F32)
                    t_dr = Trk()
                    k.op(DVE, [t_route], [t_dr], lambda h: h.tensor_copy(out=dr[:, 0:32], in_=slot_i[:]))
                    k.op(DVE, [t_route], [t_dr], lambda h: h.tensor_copy(out=dr[:, 32:64], in_=wts[:]))
                    k.dma(SP, [t_dr], [T_OUT], lambda h: h.dma_start(out=dbg_route, in_=dr[:]))
                k.barrier()
                if STOP == 2:
                    k.finish()
                    return nc

            with contextlib.ExitStack() as st:
                xe = [sb(st, f"xe{i}", [128, D], BF16) for i in range(2)]
                T_xe = [Trk() for _ in range(2)]
                xeT = [sb(st, f"xeT{i}", [128, 16, 128], BF16) for i in range(2)]
                T_xeT = [Trk(multi=True) for _ in range(2)]
                sg = sb(st, "sg", [128, 512], F32)
                T_sg2 = Trk()
                hb = sb(st, "hb", [128, 512], BF16)
                T_hb = Trk()
                hbT = sb(st, "hbT", [128, 4, 128], BF16)
                T_hbT = Trk()
                yo = [sb(st, f"yo{i}", [128, D], F32) for i in range(2)]
                T_yo = [Trk(multi=True) for _ in range(2)]
                for e in range(NE):
                    r = e % 2
                    k.dma(SP, [T_XS], [T_xe[r]], lambda h, r=r, e=e: h.dma_start(out=xe[r][:], in_=xs_scr[e * CAP:(e + 1) * CAP, :]))
                    T_xeT[r].w = {}
                    for half in range(2):
                        k.op(PE, [T_xe[r], t_const], [PSB[half]], [
                            (lambda h, r=r, c=c, half=half: h.transpose(out=psb[:, half, (c % 8) * 128:(c % 8 + 1) * 128],
                                                                       in_=xe[r][:, c * 128:(c + 1) * 128], identity=ident_b[:, :]))
                            for c in range(half * 8, half * 8 + 8)])
                        evac([PSB[half]], [T_xeT[r]], xeT[r][:, half * 8:half * 8 + 8, :],
                             psb[:, half, :].rearrange("p (c t) -> p c t", t=128))
                    for j in range(2):
                        k.op(PE, [T_xeT[r], T_ew[r][j]], [PS[j]], [
                            (lambda h, r=r, j=j, kc=kc: h.matmul(ps[:, j, :], lhsT=xeT[r][:, kc, :], rhs=ew[r][j][:, kc, :],
                                                               start=(kc == 0), stop=(kc == 15)))
                            for kc in range(16)])
                    k.op(ACT, [PS[0]], [T_sg2], lambda h: h.activation(out=sg[:], in_=ps[:, 0, :], func=AF.Silu))
                    k.op(DVE, [PS[1], T_sg2], [T_hb], lambda h: h.tensor_tensor(out=hb[:], in0=ps[:, 1, :], in1=sg[:], op=ALU.mult))
                    k.op(PE, [T_hb, t_const], [PSB[0]], [
                        (lambda h, c=c: h.transpose(out=psb[:, 0, c * 128:(c + 1) * 128], in_=hb[:, c * 128:(c + 1) * 128],
                                                    identity=ident_b[:, :]))
                        for c in range(4)])
                    k.op(DVE, [PSB[0]], [T_hbT], lambda h: h.tensor_copy(
                        out=hbT[:], in_=psb[:, 0, 0:512].rearrange("p (c t) -> p c t", t=128)))
                    wd = ew[r][2][:].rearrange("p (fc a) f -> p fc (a f)", a=4)
                    T_yo[r].w = {}
                    for ds in range(4):
                        b = 2 + ds
                        k.op(PE, [T_hbT, T_ew[r][2]], [PS[b]], [
                            (lambda h, fc=fc, b=b, ds=ds, wd=wd: h.matmul(ps[:, b, :], lhsT=hbT[:, fc, :], rhs=wd[:, fc, ds * 512:(ds + 1) * 512],
                                                                         start=(fc == 0), stop=(fc == 3)))
                            for fc in range(4)])
                        evac([PS[b]], [T_yo[r]], yo[r][:, ds * 512:(ds + 1) * 512], ps[:, b, :])
                    k.dma(SP, [T_yo[r]], [T_YS], lambda h, r=r, e=e: h.dma_start(out=y_scr[e * CAP:(e + 1) * CAP, :], in_=yo[r][:]))
                    if e + 2 < NE:
                        load_expert(e + 2)
                k.barrier()
                if STOP == 3:
                    k.finish()
                    return nc
        k.barrier()

        with contextlib.ExitStack() as st:
            gfin = sb(st, "gfin", [128, D], F32)
            t_gf = Trk()
            k.dma(SP, [], [t_gf], lambda h: h.dma_start(out=gfin[:], in_=gfin_rep))
            hh = [sb(st, f"fh{i}", [128, D], F32) for i in range(2)]
            T_hh = [Trk() for _ in range(2)]
            y1 = [sb(st, f"y1{i}", [128, D], F32) for i in range(2)]
            T_y1 = [Trk() for _ in range(2)]
            y2 = [sb(st, f"y2{i}", [128, D], F32) for i in range(2)]
            T_y2 = [Trk() for _ in range(2)]
            ot = [sb(st, f"ot{i}", [128, D], F32) for i in range(2)]
            T_ot = [Trk() for _ in range(2)]
            junk = sb(st, "junk3", [128, D], BF16)
            t_junk = Trk()
            ssb = [sb(st, f"ssf{i}", [128, 4], F32) for i in range(2)]
            T_ss = [Trk() for _ in range(2)]
            for tb in range(16):
                i = tb % 2
                k.dma(SP, [T_HS], [T_hh[i]], lambda h, i=i, tb=tb: h.dma_start(out=hh[i][:], in_=h_scr[tb * 128:(tb + 1) * 128, :]))
                for j, (yy, ty) in enumerate(((y1, T_y1), (y2, T_y2))):
                    k.dma(POOL, [T_YS, t_route], [ty[i]], lambda h, i=i, tb=tb, j=j, yy=yy: h.indirect_dma_start(
                        out=yy[i][:, :], out_offset=None, in_=y_scr[:, :],
                        in_offset=bass.IndirectOffsetOnAxis(ap=slot_i[:, 16 * j + tb:16 * j + tb + 1], axis=0)))
                k.op(DVE, [T_y1[i], T_hh[i], t_route], [T_hh[i]], lambda h, i=i, tb=tb: h.scalar_tensor_tensor(
                    out=hh[i][:], in0=y1[i][:], scalar=wts[:, tb:tb + 1], in1=hh[i][:], op0=ALU.mult, op1=ALU.add))
                k.op(DVE, [T_y2[i], T_hh[i], t_route], [T_hh[i]], lambda h, i=i, tb=tb: h.scalar_tensor_tensor(
                    out=hh[i][:], in0=y2[i][:], scalar=wts[:, 16 + tb:17 + tb], in1=hh[i][:], op0=ALU.mult, op1=ALU.add))
                rstd_of(None, hh[i][:], 128, junk, ssb[i], ssb[i][:, 2:3], [T_hh[i]], t_junk, T_ss[i])
                k.op(DVE, [T_hh[i], T_ss[i], t_gf], [T_ot[i]], lambda h, i=i: h.scalar_tensor_tensor(
                    out=ot[i][:], in0=hh[i][:], scalar=ssb[i][:, 2:3], in1=gfin[:], op0=ALU.mult, op1=ALU.mult))
                k.dma(SP, [T_ot[i]], [T_OUT], lambda h, i=i, tb=tb: h.dma_start(out=out[tb * 128:(tb + 1) * 128, :], in_=ot[i][:]))
            k.barrier()
        k.finish()
    except _Stop:
        pass
    return nc


def _t5_bucket(d):
    d = np.maximum(d, 0)
    large = 16 + (np.log(np.maximum(d, 1).astype(np.float32) / np.float32(16)) / np.float32(np.log(8.0)) * np.float32(16)).astype(np.int32)
    large = np.minimum(large, 31)
    return np.where(d < 16, d, large)


def _bias_tables(rel_bias):
    rbx = np.concatenate([rel_bias.astype(np.float32), np.full((1, 16), -30000.0, np.float32)], axis=0)
    kk = np.arange(128)[:, None]
    qq = np.arange(128)[None, :]
    dist_p = qq + 128 - kk
    idx_pg = np.where(dist_p < 128, _t5_bucket(dist_p), 32)
    idx_pf = np.where(kk >= 112, idx_pg, 32)
    dist_c = qq - kk
    idx_c = np.where(dist_c >= 0, _t5_bucket(dist_c), 32)
    mrow = np.arange(128)[:, None]
    idx_mg = np.where(mrow >= 112, 31, 32) + 0 * qq
    mdist = qq + 128 - mrow
    idx_mf = np.where((mrow >= 112) & (mdist >= 128), 31, 32)

    def tab(idx):
        t = rbx[idx]
        return np.ascontiguousarray(t.transpose(0, 2, 1).reshape(idx.shape[0], 16 * 128))

    return tab(idx_pg), tab(idx_c), tab(idx_pf), tab(idx_mg), tab(idx_mf)


_CACHE = {}


def kernel(x, meta_tokens, rel_bias, norm_mix, w_in, attn_sinks, pool_mix, pool_scale, w_attn_branch,
           w_pool_branch, w_out, norm_ffn, w_router_group, b_router_group, w_router_expert, b_router_expert,
           w_gate, w_up, w_down, norm_final):
    f = lambda a: np.ascontiguousarray(np.asarray(a, dtype=np.float32))
    x = f(x)
    meta_tokens = f(meta_tokens)
    if "nc" not in _CACHE:
        _CACHE["nc"] = build_program()
    nc = _CACHE["nc"]
    pg, pc, pf, mg, mf = _bias_tables(f(rel_bias))
    rep = lambda v, n=128: np.ascontiguousarray(np.broadcast_to(f(v).reshape(1, -1), (n, f(v).size)))
    shared = {
        "meta": np.concatenate([np.zeros((112, D), np.float32), meta_tokens], axis=0),
        "w_in": f(w_in)[0],
        "pool_mix": f(pool_mix)[0],
        "w_ab": f(w_attn_branch)[0],
        "w_pb": f(w_pool_branch)[0],
        "w_out": f(w_out)[0],
        "w_rt": np.ascontiguousarray(np.concatenate([f(w_router_group)[0], f(w_router_expert)[0]], axis=1)),
        "w_gate": f(w_gate)[0][:(1 if 10 <= STOP <= 29 else NE)],
        "w_up": f(w_up)[0][:(1 if 10 <= STOP <= 29 else NE)],
        "w_down": f(w_down)[0][:(1 if 10 <= STOP <= 29 else NE)],
        "gmix_rep": rep(norm_mix),
        "gffn_rep": rep(norm_ffn),
        "gfin_rep": rep(norm_final),
        "pscale": np.ascontiguousarray(f(pool_scale).reshape(8, 128).T),
        "sinks_rep": rep(attn_sinks),
        "brt_rep": rep(np.concatenate([f(b_router_group).reshape(-1), f(b_router_expert).reshape(-1)])),
        "base_rep": rep(np.arange(64, dtype=np.float32) * CAP),
        "ident_in": np.eye(128, dtype=np.float32),
        "tri_in": np.triu(np.ones((128, 128), np.float32), k=1),
        "ones_in": np.ones((128, 128), np.float32),
        "bias_pg": pg, "bias_c": pc, "bias_mg": mg,
    }
    lead = np.concatenate([np.zeros((112, D), np.float32), meta_tokens], axis=0)
    in_maps = []
    for c in range(NCORE):
        b, j = c // 4, c % 4
        s0 = j * TOK
        halo = lead if j == 0 else x[b, s0 - 128:s0]
        m = dict(shared)
        m["xin"] = np.concatenate([halo, x[b, s0:s0 + TOK]], axis=0)
        m["bias_pf"] = pf if j == 0 else pg
        m["bias_mf"] = mf if j == 0 else mg
        in_maps.append(m)
    res = run_bass_kernel_spmd(nc, in_maps, core_ids=list(range(NCORE)))
    outp = np.empty((2, 4 * TOK, D), np.float32)
    for c in range(NCORE):
        outp[c // 4, (c % 4) * TOK:(c % 4 + 1) * TOK] = res.results[c]["out"]
    if DEBUG:
        kernel.dbg = res.results
    return outp
```

```python
import contextlib
import numpy as np
import concourse.bass as bass
import concourse.mybir as mybir
from concourse.bass_utils import run_bass_kernel_spmd

F32 = mybir.dt.float32
BF16 = mybir.dt.bfloat16
I32 = mybir.dt.int32
AF = mybir.ActivationFunctionType
ALU = mybir.AluOpType
AX = mybir.AxisListType

D = 2048
NCORE = 8
TOK = 2048
TP = 1024
NPASS = TOK // TP
TT = 128 + TP + 128
NE = 64
CAP = 128
NSLOT = NE * CAP
EPS = 1e-6
IN_W = 6400
Q0, K0, V0, U0, GA0, GP0 = 0, 1024, 1152, 1280, 2304, 4352
DEBUG = False
STOP = 0


class _Stop(Exception):
    pass


class Trk:
    __slots__ = ("w", "r", "multi")

    def __init__(self, multi=False):
        self.w = {}
        self.r = {}
        self.multi = multi


class Eng:
    def __init__(self, name, h, sem, inorder_safe=False):
        self.name = name
        self.h = h
        self.sem = sem
        self.count = 0
        self.known = {}
        self.inorder_safe = inorder_safe
        self.dsems = []
        self.dtot = []
        self.di = 0


class K:
    def __init__(self, nc, stack):
        self.nc = nc
        self.stack = stack
        self.sems = {}
        mk = lambda n: stack.enter_context(nc.semaphore(n))
        self.pe = Eng("pe", nc.tensor, mk("s_pe"), inorder_safe=True)
        self.act = Eng("act", nc.scalar, mk("s_act"))
        self.dve = Eng("dve", nc.vector, mk("s_dve"))
        self.pool = Eng("pool", nc.gpsimd, mk("s_pool"))
        self.sp = Eng("sp", nc.sync, mk("s_sp"))
        self.engs = [self.pe, self.act, self.dve, self.pool, self.sp]
        for e, n in ((self.sp, 20), (self.pool, 20)):
            for i in range(n):
                e.dsems.append(mk(f"d_{e.name}{i}"))
                e.dtot.append(0)
        self.allsems = {}
        for e in self.engs:
            self.allsems[id(e.sem)] = e.sem
            for s in e.dsems:
                self.allsems[id(s)] = s

    def _wait(self, eng, sem, val):
        if val <= 0:
            return
        key = id(sem)
        if eng.known.get(key, 0) >= val:
            return
        eng.h.wait_ge(sem, val)
        eng.known[key] = val

    def _deps(self, eng, reads, writes):
        deps = {}

        def add(d):
            for key, val in d.items():
                if deps.get(key, 0) < val:
                    deps[key] = val

        for t in reads:
            add(t.w)
        for t in writes:
            if not t.multi:
                add(t.w)
            add(t.r)
        for key, val in deps.items():
            sem = self.allsems[key]
            if sem is eng.sem and eng.inorder_safe:
                continue
            self._wait(eng, sem, val)

    def _mark(self, reads, writes, sem, val):
        key = id(sem)
        for t in reads:
            t.r[key] = max(t.r.get(key, 0), val)
        for t in writes:
            if t.multi:
                t.w[key] = max(t.w.get(key, 0), val)
            else:
                t.w = {key: val}
                t.r = {}

    def op(self, eng, reads, writes, fns):
        if not isinstance(fns, (list, tuple)):
            fns = [fns]
        self._deps(eng, reads, writes)
        ins = None
        for f in fns:
            ins = f(eng.h)
        eng.count += 1
        ins.then_inc(eng.sem, 1)
        self._mark(reads, writes, eng.sem, eng.count)

    def dma(self, eng, reads, writes, fn):
        i = eng.di
        eng.di = (i + 1) % len(eng.dsems)
        sem = eng.dsems[i]
        self._wait(eng, sem, eng.dtot[i])
        self._deps(eng, reads, writes)
        ins = fn(eng.h)
        eng.dtot[i] += 16
        ins.then_inc(sem, 16)
        self._mark(reads, writes, sem, eng.dtot[i])

    def barrier(self, pool=True):
        for e in self.engs:
            if e is self.pool and not pool:
                continue
            for o in self.engs:
                if o is not e:
                    self._wait(e, o.sem, o.count)
                for s, tot in zip(o.dsems, o.dtot):
                    self._wait(e, s, tot)

    def finish(self):
        for o in self.engs:
            self._wait(self.sp, o.sem, o.count)
            for s, tot in zip(o.dsems, o.dtot):
                self._wait(self.sp, s, tot)


def build_program():
    nc = bass.Bass("TRN2", target_bir_lowering=False)

    def din(name, shape, dt=F32):
        return nc.dram_tensor(name, list(shape), dt, kind="ExternalInput").ap()

    xin = din("xin", [128 + TOK, D])
    meta = din("meta", [128, D])
    w_in = din("w_in", [D, IN_W])
    pool_mix = din("pool_mix", [4, 256, 256])
    w_ab = din("w_ab", [1024, D])
    w_pb = din("w_pb", [1024, D])
    w_out = din("w_out", [D, D])
    w_rt = din("w_rt", [D, 72])
    ne_decl = 1 if 10 <= STOP <= 29 else NE
    w_gate = din("w_gate", [ne_decl, D, 512])
    w_up = din("w_up", [ne_decl, D, 512])
    w_down = din("w_down", [ne_decl, 512, D])
    gmix_rep = din("gmix_rep", [128, D])
    gffn_rep = din("gffn_rep", [128, D])
    gfin_rep = din("gfin_rep", [128, D])
    pscale = din("pscale", [128, 8])
    sinks_rep = din("sinks_rep", [128, 16])
    brt_rep = din("brt_rep", [128, 72])
    base_rep = din("base_rep", [128, 64])
    ident_in = din("ident_in", [128, 128])
    tri_in = din("tri_in", [128, 128])
    ones_in = din("ones_in", [128, 128])
    bias_pg = din("bias_pg", [128, 2048])
    bias_c = din("bias_c", [128, 2048])
    bias_pf = din("bias_pf", [128, 2048])
    bias_mg = din("bias_mg", [128, 2048])
    bias_mf = din("bias_mf", [128, 2048])
    out = nc.dram_tensor("out", [TOK, D], F32, kind="ExternalOutput").ap()
    sk = "ExternalOutput" if DEBUG else "Internal"
    h_scr = nc.dram_tensor("h_scr", [TOK, D], F32, kind=sk).ap()
    xs_scr = nc.dram_tensor("xs_scr", [NSLOT, D], BF16).ap()
    y_scr = nc.dram_tensor("y_scr", [NSLOT, D], F32).ap()
    if DEBUG:
        dbg_attn = nc.dram_tensor("dbg_attn", [128, 8 * TP], BF16, kind="ExternalOutput").ap()
        dbg_pool = nc.dram_tensor("dbg_pool", [128, 8 * TP], BF16, kind="ExternalOutput").ap()
        dbg_route = nc.dram_tensor("dbg_route", [128, 64], F32, kind="ExternalOutput").ap()

    try:
      with contextlib.ExitStack() as top:
        k = K(nc, top)

        def stop_at(code):
            if STOP == code:
                k.barrier()
                k.finish()
                raise _Stop()

        PE, ACT, DVE, POOL, SP = k.pe, k.act, k.dve, k.pool, k.sp

        uniq = {"n": 0}

        def sb(stack, name, shape, dt):
            uniq["n"] += 1
            return stack.enter_context(nc.sbuf_tensor(f"{name}_{uniq['n']}", list(shape), dt))

        ps = top.enter_context(nc.psum_tensor("ps", [128, 6, 512], F32))
        psb = top.enter_context(nc.psum_tensor("psb", [128, 2, 1024], BF16))
        PS = [Trk() for _ in range(6)]
        PSB = [Trk() for _ in range(2)]
        ident_f = sb(top, "ident_f", [128, 128], F32)
        ident_b = sb(top, "ident_b", [128, 128], BF16)
        tri_f = sb(top, "tri_f", [128, 128], F32)
        ones_f = sb(top, "ones_f", [128, 128], F32)
        t_const = Trk()
        slot_i = sb(top, "slot_i", [128, 32], I32)
        wts = sb(top, "wts", [128, 32], F32)
        t_route = Trk(multi=True)
        zero_b = sb(top, "zero_b", [128, D], BF16)
        t_zero = Trk()
        T_HS = Trk(multi=True)
        T_XS = Trk(multi=True)
        T_YS = Trk(multi=True)
        T_OUT = Trk(multi=True)

        k.dma(SP, [], [t_const], lambda h: h.dma_start(out=ident_f[:], in_=ident_in))
        k.dma(SP, [], [t_const], lambda h: h.dma_start(out=tri_f[:], in_=tri_in))
        k.dma(SP, [], [t_const], lambda h: h.dma_start(out=ones_f[:], in_=ones_in))
        k.dma(POOL, [], [t_const], lambda h: h.dma_start(out=ident_b[:], in_=ident_in))
        k.op(DVE, [], [t_zero], lambda h: h.memset(zero_b[:], 0.0))
        for i in range(NSLOT // 128):
            k.dma(SP, [t_zero], [T_XS], lambda h, i=i: h.dma_start(out=xs_scr[i * 128:(i + 1) * 128, :], in_=zero_b[:]))

        stop_at(10)
        rr = {"ev": 0}

        def evac(reads, writes, out_ap, in_ap, scale=None):
            rr["ev"] += 1
            if rr["ev"] % 2 == 0:
                if scale is None:
                    k.op(ACT, reads, writes, lambda h: h.activation(out=out_ap, in_=in_ap, func=AF.Copy))
                else:
                    k.op(ACT, reads, writes, lambda h: h.activation(out=out_ap, in_=in_ap, func=AF.Copy, scale=scale))
            else:
                if scale is None:
                    k.op(DVE, reads, writes, lambda h: h.tensor_copy(out=out_ap, in_=in_ap))
                else:
                    k.op(DVE, reads, writes, lambda h: h.tensor_scalar(out=out_ap, in0=in_ap, scalar1=scale, scalar2=None, op0=ALU.mult))

        def rstd_of(stack_tiles, src_ap, nparts, junk, ss, rs, reads, t_junk, t_ss):
            k.op(ACT, reads, [t_junk, t_ss], lambda h: h.activation(out=junk[0:nparts, :], in_=src_ap, func=AF.Square, accum_out=ss[0:nparts, 0:1]))
            k.op(ACT, [t_ss], [t_ss], lambda h: h.activation(out=ss[0:nparts, 1:2], in_=ss[0:nparts, 0:1], func=AF.Sqrt, bias=EPS, scale=1.0 / D))
            k.op(DVE, [t_ss], [t_ss], lambda h: h.reciprocal(out=rs[0:nparts, 0:1], in_=ss[0:nparts, 1:2]))

        with contextlib.ExitStack() as mx:
            hnT = sb(mx, "hnT", [128, 16, TT], BF16)
            T_hn = [Trk() for _ in range(10)]
            attnT = sb(mx, "attnT", [128, 8, TP], BF16)
            T_attn = Trk(multi=True)
            poolT = sb(mx, "poolT", [128, 8, TP], BF16)
            T_pool = Trk(multi=True)
            T_mrg = Trk(multi=True)
            wsl = [sb(mx, f"wsl{i}", [128, 16, 512], BF16) for i in range(4)]
            T_w = [Trk() for _ in range(4)]
            wi = {"i": 0}
            psc = sb(mx, "psc", [128, 8], F32)
            esink = sb(mx, "esink", [128, 16], F32)
            wpm = sb(mx, "wpm", [128, 8, 256], BF16)
            t_mc = Trk()
            k.dma(SP, [], [t_mc], lambda h: h.dma_start(out=psc[:], in_=pscale))
            k.dma(SP, [], [t_mc], lambda h: h.dma_start(out=esink[:], in_=sinks_rep))
            k.op(ACT, [t_mc], [t_mc], lambda h: h.activation(out=esink[:], in_=esink[:], func=AF.Exp))
            for g in range(4):
                k.dma(POOL, [], [t_mc], lambda h, g=g: h.dma_start(
                    out=wpm[:, 2 * g:2 * g + 2, :], in_=pool_mix[g].rearrange("(kc p) d -> p kc d", p=128)))

            def load_w(src_ap, nk, ncols, col_off=0):
                i = wi["i"]
                wi["i"] = (i + 1) % 4
                k.dma(POOL, [], [T_w[i]], lambda h: h.dma_start(
                    out=wsl[i][:, 0:nk, col_off:col_off + ncols], in_=src_ap.rearrange("(kc p) m -> p kc m", p=128)))
                return wsl[i], T_w[i]

            for ps_i in range(NPASS):
                r0 = ps_i * TP
                with contextlib.ExitStack() as st:
                    gmix = sb(st, "gmix", [128, D], F32)
                    t_gm = Trk()
                    k.dma(SP, [], [t_gm], lambda h: h.dma_start(out=gmix[:], in_=gmix_rep))
                    xt = [sb(st, f"xt{i}", [128, D], F32) for i in range(2)]
                    T_xt = [Trk() for _ in range(2)]
                    xn = [sb(st, f"xn{i}", [128, D], BF16) for i in range(2)]
                    T_xn = [Trk() for _ in range(2)]
                    junk = sb(st, "junk", [128, D], BF16)
                    t_junk = Trk()
                    ssb = [sb(st, f"ssb{i}", [128, 4], F32) for i in range(2)]
                    T_ss = [Trk() for _ in range(2)]
                    for tb in range(10):
                        i = tb % 2
                        np_ = 128
                        src = meta if tb == 9 else xin[r0 + tb * 128: r0 + (tb + 1) * 128, :]
                        c0 = 1152 if tb == 9 else tb * 128
                        k.dma(SP, [], [T_xt[i]], lambda h, i=i, src=src, np_=np_: h.dma_start(out=xt[i][0:np_, :], in_=src))
                        rstd_of(None, xt[i][0:np_, :], np_, junk, ssb[i], ssb[i][:, 2:3], [T_xt[i]], t_junk, T_ss[i])
                        k.op(DVE, [T_xt[i], T_ss[i], t_gm], [T_xn[i]], lambda h, i=i, np_=np_: h.scalar_tensor_tensor(
                            out=xn[i][0:np_, :], in0=xt[i][0:np_, :], scalar=ssb[i][0:np_, 2:3], in1=gmix[0:np_, :],
                            op0=ALU.mult, op1=ALU.mult))
                        for half in range(2):
                            k.op(PE, [T_xn[i], t_const], [PSB[half]], [
                                (lambda h, i=i, c=c, half=half, np_=np_: h.transpose(
                                    out=psb[:, half, (c % 8) * 128:(c % 8) * 128 + np_],
                                    in_=xn[i][0:np_, c * 128:(c + 1) * 128], identity=ident_b[0:np_, 0:np_]))
                                for c in range(half * 8, half * 8 + 8)])
                            src_ps = psb[:, half, :].rearrange("p (c t) -> p c t", t=128)[:, :, 0:np_]
                            evac([PSB[half]], [T_hn[tb]], hnT[:, half * 8:half * 8 + 8, c0:c0 + np_], src_ps)

                    k.barrier()
                    stop_at(11)

                with contextlib.ExitStack() as st:
                    qT = sb(st, "qT", [128, 8, TP], BF16)
                    T_q = Trk(multi=True)
                    bpg = sb(st, "bpg", [128, 2048], BF16)
                    bcc = sb(st, "bcc", [128, 2048], BF16)
                    bmg = sb(st, "bmg", [128, 2048], BF16)
                    t_bt = Trk()
                    k.dma(POOL, [], [t_bt], lambda h: h.dma_start(out=bpg[:], in_=(bias_pf if ps_i == 0 else bias_pg)))
                    k.dma(POOL, [], [t_bt], lambda h: h.dma_start(out=bcc[:], in_=bias_c))
                    k.dma(POOL, [], [t_bt], lambda h: h.dma_start(out=bmg[:], in_=(bias_mf if ps_i == 0 else bias_mg)))
                    kT2 = sb(st, "kT2", [128, 4, TT], BF16)
                    T_k = Trk(multi=True)
                    vtm = sb(st, "vtm", [128, 10, 2, 65], BF16)
                    T_v = Trk(multi=True)
                    k.op(DVE, [], [T_v], lambda h: h.memset(vtm[:], 1.0))
                    bank = {"i": 0}

                    def nb():
                        bank["i"] = (bank["i"] + 1) % 6
                        return bank["i"]

                    def inproj_fm(wt, tw, nk, mcol, nsl, rhs_of, rhs_trk, evac_fn):
                        for (n0, n1) in nsl:
                            b = nb()
                            k.op(PE, [tw] + rhs_trk, [PS[b]], [
                                (lambda h, kc=kc, b=b, n0=n0, n1=n1: h.matmul(
                                    ps[:, b, 0:n1 - n0], lhsT=wt[:, kc, mcol:mcol + 128], rhs=rhs_of(kc, n0, n1),
                                    start=(kc == 0), stop=(kc == nk - 1)))
                                for kc in range(nk)])
                            evac_fn(b, n0, n1)

                    hn_rhs = lambda kc, n0, n1: hnT[:, kc, n0:n1]
                    NS_ALL = [(0, 512), (512, 1024), (1024, TT)]
                    NS_MAIN = [(128, 640), (640, 1152)]
                    wt, tw = None, None
                    i = wi["i"]
                    wi["i"] = (i + 1) % 4
                    wt, tw = wsl[i], T_w[i]
                    k.op(DVE, [], [tw], lambda h: h.memset(wt[:], 0.0))
                    for g in range(2):
                        for hf in range(2):
                            m = g * 2 + hf
                            k.dma(POOL, [], [tw], lambda h, g=g, hf=hf, m=m: h.dma_start(
                                out=wt[:, :, m * 128 + hf * 64: m * 128 + hf * 64 + 64],
                                in_=w_in[:, K0 + g * 64: K0 + g * 64 + 64].rearrange("(kc p) m -> p kc m", p=128)))
                    for m in range(4):
                        inproj_fm(wt, tw, 16, m * 128, NS_ALL, hn_rhs, T_hn,
                                  lambda b, n0, n1, m=m: evac([PS[b]], [T_k], kT2[:, m, n0:n1], ps[:, b, 0:n1 - n0]))
                    wv, twv = load_w(w_in[:, V0:V0 + 128], 16, 128)
                    for tb in range(10):
                        np_ = 128
                        c0 = 1152 if tb == 9 else tb * 128
                        b = nb()
                        k.op(PE, [twv, T_hn[tb]], [PS[b]], [
                            (lambda h, kc=kc, b=b, c0=c0, np_=np_: h.matmul(
                                ps[0:np_, b, 0:128], lhsT=hnT[:, kc, c0:c0 + np_], rhs=wv[:, kc, 0:128],
                                start=(kc == 0), stop=(kc == 15)))
                            for kc in range(16)])
                        evac([PS[b]], [T_v], vtm[0:np_, tb, :, 0:64],
                             ps[0:np_, b, 0:128].rearrange("p (g d) -> p g d", g=2))
                    for qh in range(2):
                        wt, tw = load_w(w_in[:, Q0 + qh * 512: Q0 + (qh + 1) * 512], 16, 512)
                        for m in range(4):
                            c = qh * 4 + m
                            inproj_fm(wt, tw, 16, m * 128, NS_MAIN, hn_rhs, T_hn,
                                      lambda b, n0, n1, c=c: evac([PS[b]], [T_q], qT[:, c, n0 - 128:n1 - 128],
                                                                  ps[:, b, 0:n1 - n0], scale=0.125))

                    stop_at(12)
                    pts = [[sb(st, f"pt{r}_{s}", [128, 512], BF16) for s in range(3)] for r in range(2)]
                    T_pt = [[Trk() for s in range(3)] for r in range(2)]
                    scs = [sb(st, f"sc{r}", [128, 512], F32) for r in range(2)]
                    T_sc = [Trk() for r in range(2)]
                    atm = sb(st, "atm", [128, 16, 64], BF16)
                    T_atm = Trk(multi=True)
                    den = sb(st, "den", [128, 4, 8], F32)
                    T_den = [Trk() for _ in range(4)]
                    sbank = {"i": 0}
                    for b in range(TP // 128):
                        if ps_i == 0 and b == 1:
                            k.dma(POOL, [], [t_bt], lambda h: h.dma_start(out=bpg[:], in_=bias_pg))
                            k.dma(POOL, [], [t_bt], lambda h: h.dma_start(out=bmg[:], in_=bias_mg))
                        btabs = [bpg, bcc, bmg]
                        kcols = [b * 128, (b + 1) * 128, 1152]
                        vblk = [b, b + 1, 9]
                        for hq in range(4):
                            g = hq // 2
                            r = hq % 2
                            for s in range(3):
                                sbk = sbank["i"]
                                sbank["i"] = (sbank["i"] + 1) % 2
                                fns = []
                                for i in range(4):
                                    hd = hq * 4 + i
                                    c, hf = hd // 2, hd % 2
                                    fns.append(lambda h, s=s, sbk=sbk, i=i, c=c, hf=hf, g=g, b=b: h.matmul(
                                        ps[:, sbk, i * 128:(i + 1) * 128],
                                        lhsT=kT2[:, g * 2 + hf, kcols[s]:kcols[s] + 128],
                                        rhs=qT[:, c, b * 128:(b + 1) * 128],
                                        start=True, stop=True))
                                k.op(PE, [T_k, T_q], [PS[sbk]], fns)
                                k.op(DVE, [PS[sbk], t_bt], [T_sc[sbk]], lambda h, s=s, sbk=sbk, hq=hq: h.tensor_tensor(
                                    out=scs[sbk][:], in0=ps[:, sbk, :], in1=btabs[s][:, hq * 512:(hq + 1) * 512], op=ALU.add))
                                k.op(ACT, [T_sc[sbk]], [T_pt[r][s]], lambda h, r=r, s=s, sbk=sbk: h.activation(
                                    out=pts[r][s][:], in_=scs[sbk][:], func=AF.Exp))
                            if b == 0 and hq == 0:
                                stop_at(20)
                            ob = 2 + hq
                            fns = []
                            for i in range(4):
                                for s in range(3):
                                    fns.append(lambda h, i=i, s=s, r=r, g=g, ob=ob: h.matmul(
                                        ps[:, ob, i * 65:(i + 1) * 65], lhsT=pts[r][s][:, i * 128:(i + 1) * 128],
                                        rhs=vtm[:, vblk[s], g, :], start=(s == 0), stop=(s == 2)))
                            k.op(PE, [T_pt[r][0], T_pt[r][1], T_pt[r][2], T_v], [PS[ob]], fns)
                            if b == 0 and hq == 0:
                                stop_at(21)
                            ov = ps[:, ob, 0:260].rearrange("p (i d) -> p i d", d=65)
                            k.op(DVE, [PS[ob], t_mc], [T_den[hq]], lambda h, hq=hq, ov=ov: h.tensor_tensor(
                                out=den[:, hq, 0:4], in0=ov[:, :, 64], in1=esink[:, hq * 4:hq * 4 + 4], op=ALU.add))
                            k.op(DVE, [T_den[hq]], [T_den[hq]], lambda h, hq=hq: h.reciprocal(out=den[:, hq, 4:8], in_=den[:, hq, 0:4]))
                            for i in range(4):
                                k.op(DVE, [PS[ob], T_den[hq]], [T_atm], lambda h, hq=hq, i=i, ov=ov: h.tensor_scalar(
                                    out=atm[:, hq * 4 + i, :], in0=ov[:, i, 0:64], scalar1=den[:, hq, 4 + i:5 + i], scalar2=None,
                                    op0=ALU.mult))
                        if b == 0:
                            stop_at(22)
                        k.op(PE, [T_atm, t_const], [PSB[0]], [
                            (lambda h, c=c: h.transpose(out=psb[:, 0, c * 128:(c + 1) * 128],
                                                        in_=atm[:, 2 * c:2 * c + 2, :].rearrange("p a d -> p (a d)"),
                                                        identity=ident_b[:, :]))
                            for c in range(8)])
                        k.op(DVE, [PSB[0]], [T_attn], lambda h, b=b: h.tensor_copy(
                            out=attnT[:, :, b * 128:(b + 1) * 128], in_=psb[:, 0, :].rearrange("p (c t) -> p c t", t=128)))
                        T_atm.w = {}
                        if b == 0:
                            stop_at(23)
                    if DEBUG and ps_i == 0:
                        k.dma(SP, [T_attn], [T_OUT], lambda h: h.dma_start(out=dbg_attn, in_=attnT[:].rearrange("p c t -> p (c t)")))
                    k.barrier(pool=False)

                stop_at(13)
                with contextlib.ExitStack() as st:
                    ut = [sb(st, f"ut{i}", [128, 1152], F32) for i in range(2)]
                    T_u = [Trk(multi=True) for _ in range(2)]
                    ta = sb(st, "pa", [128, 1152], F32)
                    tb_ = sb(st, "pb", [128, 1152], F32)
                    T_ta, T_tb = Trk(), Trk()
                    mixT = sb(st, "mixT", [128, 8, TP], BF16)
                    T_mix = [Trk() for _ in range(8)]
                    NS_U = [(0, 512), (512, 1024), (1024, 1152)]
                    WIN = [2, 2, 4, 4, 8, 8, 16, 16]
                    bank["i"] = 0
                    for uh in range(2):
                        wt, tw = load_w(w_in[:, U0 + uh * 512: U0 + (uh + 1) * 512], 16, 512)
                        for m in range(4):
                            c = uh * 4 + m
                            ui = c % 2
                            T_u[ui].w = {}
                            inproj_fm(wt, tw, 16, m * 128, NS_U, hn_rhs, T_hn,
                                      lambda b, n0, n1, ui=ui: evac([PS[b]], [T_u[ui]], ut[ui][:, n0:n1], ps[:, b, 0:n1 - n0]))
                            u = ut[ui]
                            w = WIN[c]
                            k.op(DVE, [T_u[ui]], [T_ta], lambda h, u=u: h.tensor_tensor(
                                out=ta[:, 1:1152], in0=u[:, 1:1152], in1=u[:, 0:1151], op=ALU.add))
                            cur, tcur = ta, T_ta
                            if w >= 4:
                                k.op(DVE, [T_ta], [T_tb], lambda h: h.tensor_tensor(
                                    out=tb_[:, 3:1152], in0=ta[:, 3:1152], in1=ta[:, 1:1150], op=ALU.add))
                                cur, tcur = tb_, T_tb
                            if w >= 8:
                                k.op(DVE, [T_tb], [T_ta], lambda h: h.tensor_tensor(
                                    out=ta[:, 7:1152], in0=tb_[:, 7:1152], in1=tb_[:, 3:1148], op=ALU.add))
                                cur, tcur = ta, T_ta
                            if w >= 16:
                                k.op(DVE, [T_ta], [T_tb], lambda h: h.tensor_tensor(
                                    out=tb_[:, 15:1152], in0=ta[:, 15:1152], in1=ta[:, 7:1144], op=ALU.add))
                                cur, tcur = tb_, T_tb
                            k.op(DVE, [tcur, T_u[ui]], [T_mix[c]], lambda h, cur=cur, u=u, w=w, c=c: h.scalar_tensor_tensor(
                                out=mixT[:, c, :], in0=cur[:, 128:1152], scalar=1.0 / w, in1=u[:, 128:1152],
                                op0=ALU.mult, op1=ALU.subtract))
                            if c % 2 == 1:
                                g = c // 2
                                for dc in range(2):
                                    for (n0, n1) in [(0, 512), (512, 1024)]:
                                        b = nb()
                                        k.op(PE, [T_mix[c - 1], T_mix[c], t_mc], [PS[b]], [
                                            (lambda h, kc=kc, b=b, g=g, dc=dc, n0=n0, n1=n1: h.matmul(
                                                ps[:, b, 0:512], lhsT=wpm[:, 2 * g + kc, dc * 128:(dc + 1) * 128],
                                                rhs=mixT[:, 2 * g + kc, n0:n1], start=(kc == 0), stop=(kc == 1)))
                                            for kc in range(2)])
                                        evac([PS[b], t_mc], [T_pool], poolT[:, 2 * g + dc, n0:n1], ps[:, b, 0:512],
                                             scale=psc[:, 2 * g + dc:2 * g + dc + 1])
                    if DEBUG and ps_i == 0:
                        k.dma(SP, [T_pool], [T_OUT], lambda h: h.dma_start(out=dbg_pool, in_=poolT[:].rearrange("p c t -> p (c t)")))
                    k.barrier(pool=False)

                stop_at(14)
                with contextlib.ExitStack() as st:
                    mergedT = sb(st, "mergedT", [128, 16, TP], BF16)
                    sga = [sb(st, f"sga{i}", [128, 512], F32) for i in range(2)]
                    T_sg = [Trk() for _ in range(2)]
                    tmpa = sb(st, "tmpa", [128, 512], F32)
                    tmpp = sb(st, "tmpp", [128, 512], F32)
                    T_tma, T_tmp = Trk(), Trk()
                    at_rhs = lambda kc, n0, n1: attnT[:, kc, n0 - 128:n1 - 128]
                    pl_rhs = lambda kc, n0, n1: poolT[:, kc, n0 - 128:n1 - 128]
                    bank["i"] = 0
                    T_mrg.w = {}
                    for mg in range(4):
                        wga, tga = load_w(w_in[:, GA0 + mg * 512: GA0 + (mg + 1) * 512], 16, 512)
                        wgp, tgp = load_w(w_in[:, GP0 + mg * 512: GP0 + (mg + 1) * 512], 16, 512)
                        i = wi["i"]
                        wi["i"] = (i + 1) % 4
                        wbr, tbr = wsl[i], T_w[i]
                        k.dma(POOL, [], [tbr], lambda h, mg=mg: h.dma_start(
                            out=wbr[:, 0:8, :], in_=w_ab[:, mg * 512:(mg + 1) * 512].rearrange("(kc p) m -> p kc m", p=128)))
                        k.dma(POOL, [], [tbr], lambda h, mg=mg: h.dma_start(
                            out=wbr[:, 8:16, :], in_=w_pb[:, mg * 512:(mg + 1) * 512].rearrange("(kc p) m -> p kc m", p=128)))
                        wbr_p = wbr[:, 8:16, :]
                        for m in range(4):
                            mc = mg * 4 + m
                            for (n0, n1) in NS_MAIN:
                                def ev_sig(b, n0_, n1_, j):
                                    k.op(ACT, [PS[b]], [T_sg[j]], lambda h: h.activation(out=sga[j][:], in_=ps[:, b, :], func=AF.Sigmoid))
                                inproj_fm(wga, tga, 16, m * 128, [(n0, n1)], hn_rhs, T_hn, lambda b, a, c_: ev_sig(b, a, c_, 0))
                                inproj_fm(wbr, tbr, 8, m * 128, [(n0, n1)], at_rhs, [T_attn],
                                          lambda b, a, c_: k.op(DVE, [PS[b], T_sg[0]], [T_tma], lambda h: h.tensor_tensor(
                                              out=tmpa[:], in0=ps[:, b, :], in1=sga[0][:], op=ALU.mult)))
                                inproj_fm(wgp, tgp, 16, m * 128, [(n0, n1)], hn_rhs, T_hn, lambda b, a, c_: ev_sig(b, a, c_, 1))
                                inproj_fm(wbr_p, tbr, 8, m * 128, [(n0, n1)], pl_rhs, [T_pool],
                                          lambda b, a, c_: k.op(DVE, [PS[b], T_sg[1]], [T_tmp], lambda h: h.tensor_tensor(
                                              out=tmpp[:], in0=ps[:, b, :], in1=sga[1][:], op=ALU.mult)))
                                k.op(DVE, [T_tma, T_tmp], [T_mrg], lambda h, mc=mc, n0=n0, n1=n1: h.tensor_tensor(
                                    out=mergedT[:, mc, n0 - 128:n1 - 128], in0=tmpa[:], in1=tmpp[:], op=ALU.add))

                    stop_at(15)
                    xr = [sb(st, f"xr{i}", [128, 512], F32) for i in range(2)]
                    T_xr = [Trk() for _ in range(2)]
                    ho = [sb(st, f"ho{i}", [128, 512], F32) for i in range(2)]
                    T_ho = [Trk() for _ in range(2)]
                    bank["i"] = 0
                    cnt = 0
                    for ds in range(4):
                        wt, tw = load_w(w_out[:, ds * 512:(ds + 1) * 512], 16, 512)
                        for tb in range(TP // 128):
                            i = cnt % 2
                            cnt += 1
                            row = ps_i * TP + tb * 128
                            k.dma(SP, [], [T_xr[i]], lambda h, i=i, row=row, ds=ds: h.dma_start(
                                out=xr[i][:], in_=xin[128 + row:128 + row + 128, ds * 512:(ds + 1) * 512]))
                            b = nb()
                            k.op(PE, [tw, T_mrg], [PS[b]], [
                                (lambda h, kc=kc, b=b, tb=tb: h.matmul(
                                    ps[:, b, :], lhsT=mergedT[:, kc, tb * 128:(tb + 1) * 128], rhs=wt[:, kc, :],
                                    start=(kc == 0), stop=(kc == 15)))
                                for kc in range(16)])
                            k.op(DVE, [PS[b], T_xr[i]], [T_ho[i]], lambda h, i=i, b=b: h.tensor_tensor(
                                out=ho[i][:], in0=ps[:, b, :], in1=xr[i][:], op=ALU.add))
                            k.dma(SP, [T_ho[i]], [T_HS], lambda h, i=i, row=row, ds=ds: h.dma_start(
                                out=h_scr[row:row + 128, ds * 512:(ds + 1) * 512], in_=ho[i][:]))
                    k.barrier(pool=False)
                T_attn.w = {}
                T_pool.w = {}
            k.barrier()
        k.barrier()
        if STOP == 1:
            k.finish()
            return nc

        with contextlib.ExitStack() as moe:
            ew = [[sb(moe, f"ew{r}_{j}", [128, 16, 512], BF16) for j in range(3)] for r in range(2)]
            T_ew = [[Trk() for j in range(3)] for r in range(2)]

            def load_expert(e):
                r = e % 2
                k.dma(POOL, [], [T_ew[r][0]], lambda h: h.dma_start(
                    out=ew[r][0][:], in_=w_gate[e].rearrange("(p kc) f -> p kc f", kc=16)))
                k.dma(POOL, [], [T_ew[r][1]], lambda h: h.dma_start(
                    out=ew[r][1][:], in_=w_up[e].rearrange("(p kc) f -> p kc f", kc=16)))
                k.dma(POOL, [], [T_ew[r][2]], lambda h: h.dma_start(
                    out=ew[r][2][:].rearrange("p (fc a) f -> p fc (a f)", a=4), in_=w_down[e].rearrange("(fc p) d -> p fc d", p=128)))

            load_expert(0)
            load_expert(1)

            with contextlib.ExitStack() as st:
                gffn = sb(st, "gffn", [128, D], F32)
                wr = sb(st, "wr", [128, 16, 72], F32)
                brt = sb(st, "brt", [128, 72], F32)
                basef = sb(st, "basef", [128, 64], F32)
                carry = sb(st, "carry", [128, 64], F32)
                t_rc = Trk()
                T_carry = Trk()
                k.dma(SP, [], [t_rc], lambda h: h.dma_start(out=gffn[:], in_=gffn_rep))
                k.dma(SP, [], [t_rc], lambda h: h.dma_start(out=wr[:], in_=w_rt.rearrange("(kc p) m -> p kc m", p=128)))
                k.dma(SP, [], [t_rc], lambda h: h.dma_start(out=brt[:], in_=brt_rep))
                k.dma(SP, [], [t_rc], lambda h: h.dma_start(out=basef[:], in_=base_rep))
                k.op(DVE, [], [T_carry], lambda h: h.memset(carry[:], 0.0))
                hh = [sb(st, f"hh{i}", [128, D], F32) for i in range(2)]
                T_hh = [Trk() for _ in range(2)]
                hn2 = [sb(st, f"hn2{i}", [128, D], F32) for i in range(2)]
                T_hn2 = [Trk() for _ in range(2)]
                hnb = [sb(st, f"hnb{i}", [128, D], BF16) for i in range(2)]
                T_hnb = [Trk() for _ in range(2)]
                hT = sb(st, "hT", [128, 16, 128], F32)
                T_hT = Trk(multi=True)
                junk = sb(st, "junk2", [128, D], BF16)
                t_junk = Trk()
                ssb = [sb(st, f"ssr{i}", [128, 4], F32) for i in range(2)]
                T_ss = [Trk() for _ in range(2)]
                sm = sb(st, "sm", [128, 512], F32)
                T_sm = Trk()
                for tb in range(16):
                    i = tb % 2
                    k.dma(SP, [T_HS], [T_hh[i]], lambda h, i=i, tb=tb: h.dma_start(out=hh[i][:], in_=h_scr[tb * 128:(tb + 1) * 128, :]))
                    rstd_of(None, hh[i][:], 128, junk, ssb[i], ssb[i][:, 2:3], [T_hh[i]], t_junk, T_ss[i])
                    k.op(DVE, [T_hh[i], T_ss[i], t_rc], [T_hn2[i]], lambda h, i=i: h.scalar_tensor_tensor(
                        out=hn2[i][:], in0=hh[i][:], scalar=ssb[i][:, 2:3], in1=gffn[:], op0=ALU.mult, op1=ALU.mult))
                    k.op(ACT, [T_hn2[i]], [T_hnb[i]], lambda h, i=i: h.activation(
                        out=hnb[i][:].rearrange("s (kc p) -> s kc p", p=128),
                        in_=hn2[i][:].rearrange("s (p kc) -> s kc p", kc=16), func=AF.Copy))
                    T_hT.w = {}
                    for q4 in range(4):
                        k.op(PE, [T_hn2[i], t_const], [PS[q4]], [
                            (lambda h, i=i, c=c, q4=q4: h.transpose(out=ps[:, q4, (c % 4) * 128:(c % 4 + 1) * 128],
                                                                   in_=hn2[i][:, c * 128:(c + 1) * 128], identity=ident_f[:, :]))
                            for c in range(q4 * 4, q4 * 4 + 4)])
                        evac([PS[q4]], [T_hT], hT[:, q4 * 4:q4 * 4 + 4, :], ps[:, q4, :].rearrange("p (c t) -> p c t", t=128))
                    k.op(PE, [T_hT, t_rc], [PS[4]], [
                        (lambda h, c=c: h.matmul(ps[:, 4, 0:72], lhsT=hT[:, c, :], rhs=wr[:, c, :], start=(c == 0), stop=(c == 15)))
                        for c in range(16)])
                    LG, OHG, ESEL, OH1, ES2, OH2, E1, E2, AA, POS, TMP = 0, 72, 80, 88, 96, 104, 112, 176, 240, 304, 368
                    GM, NGM, SUMG, PG, M1, M2, D21, W1c, W2c, S1, S2 = 440, 441, 442, 443, 444, 445, 446, 447, 448, 449, 450
                    EG = 456
                    c1 = lambda a: sm[:, a:a + 1]
                    c8 = lambda a: sm[:, a:a + 8]
                    c64 = lambda a: sm[:, a:a + 64]
                    R = [T_sm]
                    W = [T_sm]
                    k.op(DVE, [PS[4], t_rc, T_sm], W, lambda h: h.tensor_tensor(out=sm[:, LG:LG + 72], in0=ps[:, 4, 0:72], in1=brt[:], op=ALU.add))
                    k.op(DVE, R, W, lambda h: h.tensor_reduce(out=c1(GM), in_=c8(LG), axis=AX.X, op=ALU.max))
                    k.op(DVE, R, W, lambda h: h.tensor_scalar(out=c8(OHG), in0=c8(LG), scalar1=c1(GM), scalar2=None, op0=ALU.is_equal))
                    k.op(DVE, R, W, lambda h: h.tensor_scalar(out=c1(NGM), in0=c1(GM), scalar1=-1.0, scalar2=None, op0=ALU.mult))
                    k.op(ACT, R, W, lambda h: h.activation(out=c8(EG), in_=c8(LG), func=AF.Exp, bias=c1(NGM), accum_out=c1(SUMG)))
                    k.op(DVE, R, W, lambda h: h.reciprocal(out=c1(PG), in_=c1(SUMG)))
                    k.op(DVE, R, W, lambda h: h.tensor_scalar(out=c8(ESEL), in0=sm[:, LG + 8:LG + 16], scalar1=c1(OHG), scalar2=None, op0=ALU.mult))
                    for g in range(1, 8):
                        k.op(DVE, R, W, lambda h, g=g: h.scalar_tensor_tensor(
                            out=c8(ESEL), in0=sm[:, LG + 8 + g * 8:LG + 16 + g * 8], scalar=c1(OHG + g), in1=c8(ESEL),
                            op0=ALU.mult, op1=ALU.add))
                    k.op(DVE, R, W, lambda h: h.tensor_reduce(out=c1(M1), in_=c8(ESEL), axis=AX.X, op=ALU.max))
                    k.op(DVE, R, W, lambda h: h.tensor_scalar(out=c8(OH1), in0=c8(ESEL), scalar1=c1(M1), scalar2=None, op0=ALU.is_equal))
                    k.op(DVE, R, W, lambda h: h.scalar_tensor_tensor(out=c8(ES2), in0=c8(OH1), scalar=-1.0e30, in1=c8(ESEL),
                                                                   op0=ALU.mult, op1=ALU.add))
                    k.op(DVE, R, W, lambda h: h.tensor_reduce(out=c1(M2), in_=c8(ES2), axis=AX.X, op=ALU.max))
                    k.op(DVE, R, W, lambda h: h.tensor_scalar(out=c8(OH2), in0=c8(ES2), scalar1=c1(M2), scalar2=None, op0=ALU.is_equal))
                    k.op(DVE, R, W, lambda h: h.tensor_tensor(out=c1(D21), in0=c1(M2), in1=c1(M1), op=ALU.subtract))
                    k.op(ACT, R, W, lambda h: h.activation(out=c1(D21), in_=c1(D21), func=AF.Exp))
                    k.op(DVE, R, W, lambda h: h.tensor_scalar(out=c1(D21), in0=c1(D21), scalar1=1.0, scalar2=None, op0=ALU.add))
                    k.op(DVE, R, W, lambda h: h.reciprocal(out=c1(D21), in_=c1(D21)))
                    k.op(DVE, R + [t_route], W + [t_route], lambda h, tb=tb: h.tensor_tensor(out=wts[:, tb:tb + 1], in0=c1(PG), in1=c1(D21), op=ALU.mult))
                    k.op(DVE, R + [t_route], W + [t_route], lambda h, tb=tb: h.tensor_tensor(out=wts[:, 16 + tb:17 + tb], in0=c1(PG), in1=wts[:, tb:tb + 1], op=ALU.subtract))
                    for g in range(8):
                        k.op(DVE, R, W, lambda h, g=g: h.tensor_scalar(out=sm[:, E1 + g * 8:E1 + g * 8 + 8], in0=c8(OH1), scalar1=c1(OHG + g), scalar2=None, op0=ALU.mult))
                        k.op(DVE, R, W, lambda h, g=g: h.tensor_scalar(out=sm[:, E2 + g * 8:E2 + g * 8 + 8], in0=c8(OH2), scalar1=c1(OHG + g), scalar2=None, op0=ALU.mult))
                    k.op(DVE, R, W, lambda h: h.tensor_tensor(out=c64(AA), in0=c64(E1), in1=c64(E2), op=ALU.add))
                    k.op(PE, [T_sm, t_const], [PS[5]], [
                        lambda h: h.matmul(ps[:, 5, 0:64], lhsT=tri_f[:, :], rhs=c64(AA), start=True, stop=True),
                        lambda h: h.matmul(ps[:, 5, 64:128], lhsT=ones_f[:, :], rhs=c64(AA), start=True, stop=True)])
                    k.op(DVE, [PS[5], T_carry, T_sm], W, lambda h: h.tensor_tensor(out=c64(POS), in0=ps[:, 5, 0:64], in1=carry[:], op=ALU.add))
                    k.op(DVE, [PS[5], T_carry], [T_carry], lambda h: h.tensor_tensor(out=carry[:], in0=ps[:, 5, 64:128], in1=carry[:], op=ALU.add))
                    k.op(DVE, R + [t_rc], W, lambda h: h.scalar_tensor_tensor(out=c64(POS), in0=c64(POS), scalar=float(CAP - 1), in1=basef[:],
                                                                            op0=ALU.min, op1=ALU.add))
                    k.op(DVE, R, W, lambda h: h.tensor_tensor(out=c64(TMP), in0=c64(POS), in1=c64(E1), op=ALU.mult))
                    k.op(DVE, R, W, lambda h: h.tensor_reduce(out=c1(S1), in_=c64(TMP), axis=AX.X, op=ALU.add))
                    k.op(DVE, R, W, lambda h: h.tensor_tensor(out=c64(TMP), in0=c64(POS), in1=c64(E2), op=ALU.mult))
                    k.op(DVE, R, W, lambda h: h.tensor_reduce(out=c1(S2), in_=c64(TMP), axis=AX.X, op=ALU.add))
                    k.op(DVE, R + [t_route], W + [t_route], lambda h, tb=tb: h.tensor_copy(out=slot_i[:, tb:tb + 1], in_=c1(S1)))
                    k.op(DVE, R + [t_route], W + [t_route], lambda h, tb=tb: h.tensor_copy(out=slot_i[:, 16 + tb:17 + tb], in_=c1(S2)))
                    for j in range(2):
                        k.dma(POOL, [T_hnb[i], t_route], [T_XS], lambda h, i=i, tb=tb, j=j: h.indirect_dma_start(
                            out=xs_scr[:, :], out_offset=bass.IndirectOffsetOnAxis(ap=slot_i[:, 16 * j + tb:16 * j + tb + 1], axis=0),
                            in_=hnb[i][:, :], in_offset=None))
                if DEBUG:
                    dr = sb(st, "dr", [128, 64], F32)
                    t_dr = Trk()
                    k.op(DVE, [t_route], [t_dr], lambda h: h.tensor_copy(out=dr[:, 0:32], in_=slot_i[:]))
                    k.op(DVE, [t_route], [t_dr], lambda h: h.tensor_copy(out=dr[:, 32:64], in_=wts[:]))
                    k.dma(SP, [t_dr], [T_OUT], lambda h: h.dma_start(out=dbg_route, in_=dr[:]))
                k.barrier()
                if STOP == 2:
                    k.finish()
                    return nc

            with contextlib.ExitStack() as st:
                xe = [sb(st, f"xe{i}", [128, D], BF16) for i in range(2)]
                T_xe = [Trk() for _ in range(2)]
                xeT = [sb(st, f"xeT{i}", [128, 16, 128], BF16) for i in range(2)]
                T_xeT = [Trk(multi=True) for _ in range(2)]
                sg = sb(st, "sg", [128, 512], F32)
                T_sg2 = Trk()
                hb = sb(st, "hb", [128, 512], BF16)
                T_hb = Trk()
                hbT = sb(st, "hbT", [128, 4, 128], BF16)
                T_hbT = Trk()
                yo = [sb(st, f"yo{i}", [128, D], F32) for i in range(2)]
                T_yo = [Trk(multi=True) for _ in range(2)]
                for e in range(NE):
                    r = e % 2
                    k.dma(SP, [T_XS], [T_xe[r]], lambda h, r=r, e=e: h.dma_start(out=xe[r][:], in_=xs_scr[e * CAP:(e + 1) * CAP, :]))
                    T_xeT[r].w = {}
                    for half in range(2):
                        k.op(PE, [T_xe[r], t_const], [PSB[half]], [
                            (lambda h, r=r, c=c, half=half: h.transpose(out=psb[:, half, (c % 8) * 128:(c % 8 + 1) * 128],
                                                                       in_=xe[r][:, c * 128:(c + 1) * 128], identity=ident_b[:, :]))
                            for c in range(half * 8, half * 8 + 8)])
                        evac([PSB[half]], [T_xeT[r]], xeT[r][:, half * 8:half * 8 + 8, :],
                             psb[:, half, :].rearrange("p (c t) -> p c t", t=128))
                    for j in range(2):
                        k.op(PE, [T_xeT[r], T_ew[r][j]], [PS[j]], [
                            (lambda h, r=r, j=j, kc=kc: h.matmul(ps[:, j, :], lhsT=xeT[r][:, kc, :], rhs=ew[r][j][:, kc, :],
                                                               start=(kc == 0), stop=(kc == 15)))
                            for kc in range(16)])
                    k.op(ACT, [PS[0]], [T_sg2], lambda h: h.activation(out=sg[:], in_=ps[:, 0, :], func=AF.Silu))
                    k.op(DVE, [PS[1], T_sg2], [T_hb], lambda h: h.tensor_tensor(out=hb[:], in0=ps[:, 1, :], in1=sg[:], op=ALU.mult))
                    k.op(PE, [T_hb, t_const], [PSB[0]], [
                        (lambda h, c=c: h.transpose(out=psb[:, 0, c * 128:(c + 1) * 128], in_=hb[:, c * 128:(c + 1) * 128],
                                                    identity=ident_b[:, :]))
                        for c in range(4)])
                    k.op(DVE, [PSB[0]], [T_hbT], lambda h: h.tensor_copy(
                        out=hbT[:], in_=psb[:, 0, 0:512].rearrange("p (c t) -> p c t", t=128)))
                    wd = ew[r][2][:].rearrange("p (fc a) f -> p fc (a f)", a=4)
                    T_yo[r].w = {}
                    for ds in range(4):
                        b = 2 + ds
                        k.op(PE, [T_hbT, T_ew[r][2]], [PS[b]], [
                            (lambda h, fc=fc, b=b, ds=ds, wd=wd: h.matmul(ps[:, b, :], lhsT=hbT[:, fc, :], rhs=wd[:, fc, ds * 512:(ds + 1) * 512],
                                                                         start=(fc == 0), stop=(fc == 3)))
                            for fc in range(4)])
                        evac([PS[b]], [T_yo[r]], yo[r][:, ds * 512:(ds + 1) * 512], ps[:, b, :])
                    k.dma(SP, [T_yo[r]], [T_YS], lambda h, r=r, e=e: h.dma_start(out=y_scr[e * CAP:(e + 1) * CAP, :], in_=yo[r][:]))
                    if e + 2 < NE:
                        load_expert(e + 2)
                k.barrier()
                if STOP == 3:
                    k.finish()
                    return nc
        k.barrier()

        with contextlib.ExitStack() as st:
            gfin = sb(st, "gfin", [128, D], F32)
            t_gf = Trk()
            k.dma(SP, [], [t_gf], lambda h: h.dma_start(out=gfin[:], in_=gfin_rep))
            hh = [sb(st, f"fh{i}", [128, D], F32) for i in range(2)]
            T_hh = [Trk() for _ in range(2)]
            y1 = [sb(st, f"y1{i}", [128, D], F32) for i in range(2)]
            T_y1 = [Trk() for _ in range(2)]
            y2 = [sb(st, f"y2{i}", [128, D], F32) for i in range(2)]
            T_y2 = [Trk() for _ in range(2)]
            ot = [sb(st, f"ot{i}", [128, D], F32) for i in range(2)]
            T_ot = [Trk() for _ in range(2)]
            junk = sb(st, "junk3", [128, D], BF16)
            t_junk = Trk()
            ssb = [sb(st, f"ssf{i}", [128, 4], F32) for i in range(2)]
            T_ss = [Trk() for _ in range(2)]
            for tb in range(16):
                i = tb % 2
                k.dma(SP, [T_HS], [T_hh[i]], lambda h, i=i, tb=tb: h.dma_start(out=hh[i][:], in_=h_scr[tb * 128:(tb + 1) * 128, :]))
                for j, (yy, ty) in enumerate(((y1, T_y1), (y2, T_y2))):
                    k.dma(POOL, [T_YS, t_route], [ty[i]], lambda h, i=i, tb=tb, j=j, yy=yy: h.indirect_dma_start(
                        out=yy[i][:, :], out_offset=None, in_=y_scr[:, :],
                        in_offset=bass.IndirectOffsetOnAxis(ap=slot_i[:, 16 * j + tb:16 * j + tb + 1], axis=0)))
                k.op(DVE, [T_y1[i], T_hh[i], t_route], [T_hh[i]], lambda h, i=i, tb=tb: h.scalar_tensor_tensor(
                    out=hh[i][:], in0=y1[i][:], scalar=wts[:, tb:tb + 1], in1=hh[i][:], op0=ALU.mult, op1=ALU.add))
                k.op(DVE, [T_y2[i], T_hh[i], t_route], [T_hh[i]], lambda h, i=i, tb=tb: h.scalar_tensor_tensor(
                    out=hh[i][:], in0=y2[i][:], scalar=wts[:, 16 + tb:17 + tb], in1=hh[i][:], op0=ALU.mult, op1=ALU.add))
                rstd_of(None, hh[i][:], 128, junk, ssb[i], ssb[i][:, 2:3], [T_hh[i]], t_junk, T_ss[i])
                k.op(DVE, [T_hh[i], T_ss[i], t_gf], [T_ot[i]], lambda h, i=i: h.scalar_tensor_tensor(
                    out=ot[i][:], in0=hh[i][:], scalar=ssb[i][:, 2:3], in1=gfin[:], op0=ALU.mult, op1=ALU.mult))
                k.dma(SP, [T_ot[i]], [T_OUT], lambda h, i=i, tb=tb: h.dma_start(out=out[tb * 128:(tb + 1) * 128, :], in_=ot[i][:]))
            k.barrier()
        k.finish()
    except _Stop:
        pass
    return nc


def _t5_bucket(d):
    d = np.maximum(d, 0)
    large = 16 + (np.log(np.maximum(d, 1).astype(np.float32) / np.float32(16)) / np.float32(np.log(8.0)) * np.float32(16)).astype(np.int32)
    large = np.minimum(large, 31)
    return np.where(d < 16, d, large)


def _bias_tables(rel_bias):
    rbx = np.concatenate([rel_bias.astype(np.float32), np.full((1, 16), -30000.0, np.float32)], axis=0)
    kk = np.arange(128)[:, None]
    qq = np.arange(128)[None, :]
    dist_p = qq + 128 - kk
    idx_pg = np.where(dist_p < 128, _t5_bucket(dist_p), 32)
    idx_pf = np.where(kk >= 112, idx_pg, 32)
    dist_c = qq - kk
    idx_c = np.where(dist_c >= 0, _t5_bucket(dist_c), 32)
    mrow = np.arange(128)[:, None]
    idx_mg = np.where(mrow >= 112, 31, 32) + 0 * qq
    mdist = qq + 128 - mrow
    idx_mf = np.where((mrow >= 112) & (mdist >= 128), 31, 32)

    def tab(idx):
        t = rbx[idx]
        return np.ascontiguousarray(t.transpose(0, 2, 1).reshape(idx.shape[0], 16 * 128))

    return tab(idx_pg), tab(idx_c), tab(idx_pf), tab(idx_mg), tab(idx_mf)


_CACHE = {}


def kernel(x, meta_tokens, rel_bias, norm_mix, w_in, attn_sinks, pool_mix, pool_scale, w_attn_branch,
           w_pool_branch, w_out, norm_ffn, w_router_group, b_router_group, w_router_expert, b_router_expert,
           w_gate, w_up, w_down, norm_final):
    f = lambda a: np.ascontiguousarray(np.asarray(a, dtype=np.float32))
    x = f(x)
    meta_tokens = f(meta_tokens)
    if "nc" not in _CACHE:
        _CACHE["nc"] = build_program()
    nc = _CACHE["nc"]
    pg, pc, pf, mg, mf = _bias_tables(f(rel_bias))
    rep = lambda v, n=128: np.ascontiguousarray(np.broadcast_to(f(v).reshape(1, -1), (n, f(v).size)))
    shared = {
        "meta": np.concatenate([np.zeros((112, D), np.float32), meta_tokens], axis=0),
        "w_in": f(w_in)[0],
        "pool_mix": f(pool_mix)[0],
        "w_ab": f(w_attn_branch)[0],
        "w_pb": f(w_pool_branch)[0],
        "w_out": f(w_out)[0],
        "w_rt": np.ascontiguousarray(np.concatenate([f(w_router_group)[0], f(w_router_expert)[0]], axis=1)),
        "w_gate": f(w_gate)[0][:(1 if 10 <= STOP <= 29 else NE)],
        "w_up": f(w_up)[0][:(1 if 10 <= STOP <= 29 else NE)],
        "w_down": f(w_down)[0][:(1 if 10 <= STOP <= 29 else NE)],
        "gmix_rep": rep(norm_mix),
        "gffn_rep": rep(norm_ffn),
        "gfin_rep": rep(norm_final),
        "pscale": np.ascontiguousarray(f(pool_scale).reshape(8, 128).T),
        "sinks_rep": rep(attn_sinks),
        "brt_rep": rep(np.concatenate([f(b_router_group).reshape(-1), f(b_router_expert).reshape(-1)])),
        "base_rep": rep(np.arange(64, dtype=np.float32) * CAP),
        "ident_in": np.eye(128, dtype=np.float32),
        "tri_in": np.triu(np.ones((128, 128), np.float32), k=1),
        "ones_in": np.ones((128, 128), np.float32),
        "bias_pg": pg, "bias_c": pc, "bias_mg": mg,
    }
    lead = np.concatenate([np.zeros((112, D), np.float32), meta_tokens], axis=0)
    in_maps = []
    for c in range(NCORE):
        b, j = c // 4, c % 4
        s0 = j * TOK
        halo = lead if j == 0 else x[b, s0 - 128:s0]
        m = dict(shared)
        m["xin"] = np.concatenate([halo, x[b, s0:s0 + TOK]], axis=0)
        m["bias_pf"] = pf if j == 0 else pg
        m["bias_mf"] = mf if j == 0 else mg
        in_maps.append(m)
    res = run_bass_kernel_spmd(nc, in_maps, core_ids=list(range(NCORE)))
    outp = np.empty((2, 4 * TOK, D), np.float32)
    for c in range(NCORE):
        outp[c // 4, (c % 4) * TOK:(c % 4 + 1) * TOK] = res.results[c]["out"]
    if DEBUG:
        kernel.dbg = res.results
    return outp
```
